# Optimizing a Trainium2 kernel written in Bass

```python
import math
import jax, jax.numpy as jnp
from jax import lax
import numpy as np

D_MODEL = 1024
BATCH = 8
SEQ = 4096
DEPTH = 2

CHUNK = 64
EPS = 1e-6
SSD_HEADS = 16
SSD_HEAD_DIM = 64
SSD_INNER = SSD_HEADS * SSD_HEAD_DIM
SSD_GROUPS = 2
HEADS_PER_GROUP = SSD_HEADS // SSD_GROUPS
SSD_STATE = 128
SSD_CONV = 4
SSD_CONV_DIM = SSD_INNER + 2 * SSD_GROUPS * SSD_STATE
ATT_HEADS = 8
ATT_QK_DIM = 64
ATT_V_DIM = 2 * ATT_QK_DIM
ATT_QK_WIDTH = ATT_HEADS * 2 * ATT_QK_DIM
ATT_V_WIDTH = ATT_HEADS * ATT_V_DIM
ROPE_THETA = 500000.0
ROPE_DIM = ATT_QK_DIM // 4
Q_BLOCK = 128
N_BRANCHES = 2
IN_COLS = SSD_INNER + SSD_CONV_DIM + SSD_HEADS + 2 * ATT_QK_WIDTH + ATT_V_WIDTH + N_BRANCHES * D_MODEL
D_FF = ((8 * D_MODEL // 3 + 127) // 128) * 128
N_EXPERTS = 8
TOP_K = 2
D_FF_EXPERT = D_FF
N_DENSE = (DEPTH + 1) // 2
N_MOE = DEPTH // 2

kernel_name = "hybrid_ssd_diffattn_moe_trunk"


def _rms(x):
    xf = x.astype(jnp.float32)
    return (xf * lax.rsqrt(jnp.mean(xf * xf, axis=-1, keepdims=True) + EPS)).astype(x.dtype)


def rmsnorm(x, w):
    return _rms(x) * w


def rope_tables(seq_len):
    inv = ROPE_THETA ** (-jnp.arange(0, ROPE_DIM, 2, dtype=jnp.float32) / ROPE_DIM)
    ang = jnp.arange(seq_len, dtype=jnp.float32)[:, None] * inv[None, :]
    return jnp.cos(ang), jnp.sin(ang)


def apply_partial_rope(t, cos, sin):
    rot, rest = t[..., :ROPE_DIM], t[..., ROPE_DIM:]
    r1, r2 = rot[..., :ROPE_DIM // 2], rot[..., ROPE_DIM // 2:]
    c = cos[None, :, None, None, :].astype(t.dtype)
    s = sin[None, :, None, None, :].astype(t.dtype)
    return jnp.concatenate([r1 * c - r2 * s, r2 * c + r1 * s, rest], axis=-1)


def causal_dwconv(u, w, b):
    out = lax.conv_general_dilated(
        u, w[:, None, :].astype(u.dtype), window_strides=(1,), padding=[(SSD_CONV - 1, 0)],
        dimension_numbers=("NWC", "WIO", "NWC"), feature_group_count=u.shape[-1])
    return out + b


def ssd_mixer(xbc, dt_raw, z, conv_w, conv_b, dt_bias, a_log, d_skip, norm_w):
    bsz, s_len, _ = xbc.shape
    nc = s_len // CHUNK
    xbc = jax.nn.silu(causal_dwconv(xbc, conv_w, conv_b))
    gn = SSD_GROUPS * SSD_STATE
    xs, bm, cm = jnp.split(xbc, [SSD_INNER, SSD_INNER + gn], axis=-1)
    xs = xs.reshape(bsz, nc, CHUNK, SSD_GROUPS, HEADS_PER_GROUP, SSD_HEAD_DIM)
    bm = bm.reshape(bsz, nc, CHUNK, SSD_GROUPS, SSD_STATE)
    cm = cm.reshape(bsz, nc, CHUNK, SSD_GROUPS, SSD_STATE)
    dt = jax.nn.softplus(dt_raw.astype(jnp.float32) + dt_bias.astype(jnp.float32))
    a = -jnp.exp(a_log.astype(jnp.float32))
    dt_c = dt.reshape(bsz, nc, CHUNK, SSD_GROUPS, HEADS_PER_GROUP)
    adt = jnp.moveaxis((dt * a).reshape(bsz, nc, CHUNK, SSD_GROUPS, HEADS_PER_GROUP), 2, -1)
    a_cs = jnp.cumsum(adt, axis=-1)
    xdt = xs * dt_c[..., None].astype(xs.dtype)
    seg = a_cs[..., :, None] - a_cs[..., None, :]
    causal = jnp.tril(jnp.ones((CHUNK, CHUNK), dtype=bool))
    decay_in = jnp.exp(jnp.where(causal, seg, -jnp.inf))
    cb = jnp.einsum('bclgn,bcsgn->bcgls', cm, bm)
    y_diag = jnp.einsum('bcgels,bcsgep->bclgep', cb[:, :, :, None] * decay_in, xdt)
    decay_to_end = jnp.exp(a_cs[..., -1:] - a_cs)
    states = jnp.einsum('bclgn,bcgel,bclgep->bcgepn', bm, decay_to_end, xdt)
    chunk_decay = jnp.exp(a_cs[..., -1])

    def step(h, inp):
        s_c, d_c = inp
        h_new = (d_c[..., None, None] * h + s_c).astype(h.dtype)
        return h_new, h

    h0 = jnp.zeros_like(states[:, 0])
    _, prev = lax.scan(step, h0, (jnp.moveaxis(states, 1, 0), jnp.moveaxis(chunk_decay, 1, 0)))
    prev = jnp.moveaxis(prev, 0, 1)
    y_off = jnp.einsum('bclgn,bcgepn,bcgel->bclgep', cm, prev, jnp.exp(a_cs))
    y = y_diag + y_off + xs * d_skip.reshape(SSD_GROUPS, HEADS_PER_GROUP)[:, :, None]
    y = y.reshape(bsz, s_len, SSD_INNER).astype(z.dtype)
    yz = (y * jax.nn.silu(z)).reshape(bsz, s_len, SSD_GROUPS, SSD_INNER // SSD_GROUPS)
    return _rms(yz).reshape(bsz, s_len, SSD_INNER) * norm_w


def diff_attention(q, k, v, qn_w, kn_w, lq1, lk1, lq2, lk2, subln_w, lambda_init, cos, sin):
    bsz, s_len, _ = q.shape
    q = q.reshape(bsz, s_len, ATT_HEADS, 2, ATT_QK_DIM)
    k = k.reshape(bsz, s_len, ATT_HEADS, 2, ATT_QK_DIM)
    v = v.reshape(bsz, s_len, ATT_HEADS, ATT_V_DIM)
    q = apply_partial_rope(rmsnorm(q, qn_w), cos, sin) * (ATT_QK_DIM ** -0.5)
    k = apply_partial_rope(rmsnorm(k, kn_w), cos, sin)
    lam = (jnp.exp(jnp.sum(lq1.astype(jnp.float32) * lk1.astype(jnp.float32)))
           - jnp.exp(jnp.sum(lq2.astype(jnp.float32) * lk2.astype(jnp.float32))) + lambda_init)
    nqb = s_len // Q_BLOCK
    qb = jnp.moveaxis(q.reshape(bsz, nqb, Q_BLOCK, ATT_HEADS, 2, ATT_QK_DIM), 1, 0)
    key_chunk = jnp.arange(s_len) // CHUNK

    def block(args):
        q_blk, i = args
        q_chunk = (i * Q_BLOCK + jnp.arange(Q_BLOCK)) // CHUNK
        allowed = key_chunk[None, :] <= q_chunk[:, None]
        sc = jnp.einsum('bqhjd,bkhjd->bhjqk', q_blk, k).astype(jnp.float32)
        p = jax.nn.softmax(jnp.where(allowed, sc, -jnp.inf), axis=-1)
        a_map = p[:, :, 0] - lam * p[:, :, 1]
        return jnp.einsum('bhqk,bkhe->bqhe', a_map.astype(v.dtype), v)

    o = lax.map(block, (qb, jnp.arange(nqb)))
    o = jnp.moveaxis(o, 0, 1).reshape(bsz, s_len, ATT_HEADS, ATT_V_DIM)
    o = rmsnorm(o, subln_w) * (1.0 - lambda_init)
    return o.reshape(bsz, s_len, ATT_V_WIDTH)


def swiglu(t, wg, wu, wd):
    return (jax.nn.silu(t @ wg) * (t @ wu)) @ wd


def moe_swiglu(h, router_w, wg, wu, wd):
    bsz, s_len, d = h.shape
    t = h.reshape(-1, d)
    logits = (t @ router_w).astype(jnp.float32)
    top_v, top_i = lax.top_k(logits, TOP_K)
    top_w = jax.nn.softmax(top_v, axis=-1)
    combine = jnp.sum(jax.nn.one_hot(top_i, N_EXPERTS, dtype=jnp.float32) * top_w[..., None], axis=1)
    out = jnp.zeros_like(t)
    for e in range(N_EXPERTS):
        out = out + combine[:, e:e + 1].astype(t.dtype) * swiglu(t, wg[e], wu[e], wd[e])
    return out.reshape(bsz, s_len, d)


def setup_inputs(seed: int = 0) -> dict:
    key = jax.random.key(seed)
    ks = jax.random.split(key, 32)
    f32 = jnp.float32
    nrm = lambda k, shape, scale: jax.random.normal(k, shape, f32) * scale
    gain = lambda k, shape: 1.0 + 0.01 * jax.random.normal(k, shape, f32)
    dt0 = jnp.exp(jax.random.uniform(ks[5], (DEPTH, SSD_HEADS), f32, math.log(1e-3), math.log(1e-1)))
    return {
        "x": nrm(ks[0], (BATCH, SEQ, D_MODEL), 1.0),
        "norm_mix_w": gain(ks[1], (DEPTH, D_MODEL)),
        "w_in": nrm(ks[2], (DEPTH, D_MODEL, IN_COLS), D_MODEL ** -0.5),
        "conv_w": nrm(ks[3], (DEPTH, SSD_CONV, SSD_CONV_DIM), SSD_CONV ** -0.5),
        "conv_b": nrm(ks[4], (DEPTH, SSD_CONV_DIM), 0.01),
        "dt_bias": dt0 + jnp.log(-jnp.expm1(-dt0)),
        "a_log": jnp.log(jax.random.uniform(ks[6], (DEPTH, SSD_HEADS), f32, 1.0, 16.0)),
        "d_skip": gain(ks[7], (DEPTH, SSD_HEADS)),
        "ssd_norm_w": gain(ks[8], (DEPTH, SSD_INNER)),
        "q_norm_w": gain(ks[9], (DEPTH, ATT_QK_DIM)),
        "k_norm_w": gain(ks[10], (DEPTH, ATT_QK_DIM)),
        "lambda_q1": nrm(ks[11], (DEPTH, ATT_QK_DIM), 0.1),
        "lambda_k1": nrm(ks[12], (DEPTH, ATT_QK_DIM), 0.1),
        "lambda_q2": nrm(ks[13], (DEPTH, ATT_QK_DIM), 0.1),
        "lambda_k2": nrm(ks[14], (DEPTH, ATT_QK_DIM), 0.1),
        "subln_w": gain(ks[15], (DEPTH, ATT_V_DIM)),
        "gate_b": nrm(ks[16], (DEPTH, N_BRANCHES, D_MODEL), 0.01),
        "w_br_ssd": nrm(ks[17], (DEPTH, SSD_INNER, D_MODEL), SSD_INNER ** -0.5),
        "w_br_att": nrm(ks[18], (DEPTH, ATT_V_WIDTH, D_MODEL), ATT_V_WIDTH ** -0.5),
        "w_out": nrm(ks[19], (DEPTH, D_MODEL, D_MODEL), D_MODEL ** -0.5),
        "norm_ffn_w": gain(ks[20], (DEPTH, D_MODEL)),
        "ffn_w_gate": nrm(ks[21], (N_DENSE, D_MODEL, D_FF), D_MODEL ** -0.5),
        "ffn_w_up": nrm(ks[22], (N_DENSE, D_MODEL, D_FF), D_MODEL ** -0.5),
        "ffn_w_down": nrm(ks[23], (N_DENSE, D_FF, D_MODEL), D_FF ** -0.5),
        "router_w": nrm(ks[24], (N_MOE, D_MODEL, N_EXPERTS), D_MODEL ** -0.5),
        "moe_w_gate": nrm(ks[25], (N_MOE, N_EXPERTS, D_MODEL, D_FF_EXPERT), D_MODEL ** -0.5),
        "moe_w_up": nrm(ks[26], (N_MOE, N_EXPERTS, D_MODEL, D_FF_EXPERT), D_MODEL ** -0.5),
        "moe_w_down": nrm(ks[27], (N_MOE, N_EXPERTS, D_FF_EXPERT, D_MODEL), D_FF_EXPERT ** -0.5),
    }


def reference(x, norm_mix_w, w_in, conv_w, conv_b, dt_bias, a_log, d_skip, ssd_norm_w,
              q_norm_w, k_norm_w, lambda_q1, lambda_k1, lambda_q2, lambda_k2, subln_w,
              gate_b, w_br_ssd, w_br_att, w_out, norm_ffn_w, ffn_w_gate, ffn_w_up, ffn_w_down,
              router_w, moe_w_gate, moe_w_up, moe_w_down):
    s_len = x.shape[1]
    cos, sin = rope_tables(s_len)
    o1 = SSD_INNER
    o2 = o1 + SSD_CONV_DIM
    o3 = o2 + SSD_HEADS
    o4 = o3 + ATT_QK_WIDTH
    o5 = o4 + ATT_QK_WIDTH
    o6 = o5 + ATT_V_WIDTH
    o7 = o6 + D_MODEL
    for i in range(DEPTH):
        lambda_init = 0.8 - 0.6 * math.exp(-0.3 * i)
        xn = rmsnorm(x, norm_mix_w[i])
        proj = xn @ w_in[i]
        z, xbc, dt_raw, q, k, v, g_s, g_a = jnp.split(proj, [o1, o2, o3, o4, o5, o6, o7], axis=-1)
        y_ssd = ssd_mixer(xbc, dt_raw, z, conv_w[i], conv_b[i], dt_bias[i], a_log[i],
                          d_skip[i], ssd_norm_w[i])
        y_att = diff_attention(q, k, v, q_norm_w[i], k_norm_w[i], lambda_q1[i], lambda_k1[i],
                               lambda_q2[i], lambda_k2[i], subln_w[i], lambda_init, cos, sin)
        merged = (jax.nn.sigmoid(g_s + gate_b[i, 0]) * (y_ssd @ w_br_ssd[i])
                  + jax.nn.sigmoid(g_a + gate_b[i, 1]) * (y_att @ w_br_att[i]))
        x = x + merged @ w_out[i]
        hn = rmsnorm(x, norm_ffn_w[i])
        j = i // 2
        if i % 2 == 0:
            x = x + swiglu(hn, ffn_w_gate[j], ffn_w_up[j], ffn_w_down[j])
        else:
            x = x + moe_swiglu(hn, router_w[j], moe_w_gate[j], moe_w_up[j], moe_w_down[j])
    return x
```

```python
import math
import numpy as np
import ml_dtypes
import concourse.bass as bass
import concourse.mybir as mybir
from concourse.bass_utils import run_bass_kernel_spmd
from contextlib import ExitStack

F32 = mybir.dt.float32
BF16 = mybir.dt.bfloat16
AF = mybir.ActivationFunctionType
ALU = mybir.AluOpType
AX = mybir.AxisListType

D = 1024
IN_COLS = 7696
DFF = 2816
NFF = DFF // 128
NEXP = 8
EPS = 1e-6
O_Z, O_XBC, O_DT, O_Q, O_K, O_V, O_GS, O_GA = 0, 1024, 2560, 2576, 3600, 4624, 5648, 6672


class Sem:
    def __init__(self, h, name, is_dma):
        self.h, self.name, self.is_dma, self.total = h, name, is_dma, 0


class Buf:
    def __init__(self, name):
        self.name = name
        self.w = {}
        self.r = {}
        self.dsem = None
        self.dram = False


class Eng:
    def __init__(self, fw, name, eng, sem, self_sync):
        self.fw, self.name, self.e, self.sem, self.self_sync = fw, name, eng, sem, self_sync
        self.waited = {}

    def _wait(self, deps):
        for s, v in deps.items():
            if s is self.sem and not self.self_sync:
                continue
            if s.is_dma:
                v = s.total
            if self.waited.get(s, 0) >= v:
                continue
            self.e.wait_ge(s.h, v)
            self.waited[s] = v
            self.fw.n_waits += 1

    @staticmethod
    def _deps(reads, writes):
        deps = {}
        for b in reads:
            for s, v in b.w.items():
                if deps.get(s, 0) < v:
                    deps[s] = v
        for b in writes:
            for d in (b.w, b.r):
                for s, v in d.items():
                    if deps.get(s, 0) < v:
                        deps[s] = v
        return deps

    def op(self, fn, reads=(), writes=()):
        self._wait(self._deps(reads, writes))
        ins = fn()
        self.sem.total += 1
        ins.then_inc(self.sem.h, 1)
        tok = self.sem.total
        for b in reads:
            b.r[self.sem] = tok
        for b in writes:
            b.w = {self.sem: tok}
            b.r = {}
        self.fw.n_ins += 1
        return ins

    def dma(self, out_ap, in_ap, reads=(), writes=(), own=None, **kw):
        (dst,) = writes
        self._wait(self._deps(reads, writes))
        if own.dsem is None:
            own.dsem = self.fw.take_dma_sem()
        s = own.dsem
        ins = self.e.dma_start(out=out_ap, in_=in_ap, **kw)
        s.total += 16
        ins.then_inc(s.h, 16)
        for b in reads:
            b.r[s] = s.total
        if dst.dram:
            dst.w[s] = s.total
        else:
            if list(dst.w.keys()) == [s]:
                dst.w[s] = s.total
            else:
                dst.w = {s: s.total}
            dst.r = {}
        self.fw.n_dma += 1
        return ins


class FW:
    def __init__(self, nc, n_dma_sems=64):
        self.nc = nc
        self.n_waits = self.n_ins = self.n_dma = 0
        self.stack = ExitStack()
        self._free_dma = []
        self._sems = []
        for i in range(n_dma_sems):
            s = Sem(self.stack.enter_context(nc.semaphore(f"dq{i}")), f"dq{i}", True)
            self._free_dma.append(s)
            self._sems.append(s)

        def mk(name, eng, self_sync):
            s = Sem(self.stack.enter_context(nc.semaphore(f"e_{name}")), name, False)
            self._sems.append(s)
            return Eng(self, name, eng, s, self_sync)
        self.pe = mk("pe", nc.tensor, False)
        self.act = mk("act", nc.scalar, True)
        self.dve = mk("dve", nc.vector, True)
        self.pool = mk("pool", nc.gpsimd, True)
        self.sp = mk("sp", nc.sync, False)
        self.engs = (self.pe, self.act, self.dve, self.pool, self.sp)

    def take_dma_sem(self):
        return self._free_dma.pop()

    def release(self, bufs):
        for b in bufs:
            if b.dsem is not None:
                self._free_dma.insert(0, b.dsem)
                b.dsem = None

    def barrier(self):
        allv = {s: s.total for s in self._sems if s.total}
        for e in self.engs:
            e._wait(allv)


def host_consts(S):
    j = np.arange(128)
    same = (j[:, None] // 64) == (j[None, :] // 64)
    V = (same & (j[:, None] <= j[None, :])).astype(np.float32)
    U = (same & (j[:, None] > j[None, :])).astype(np.float32)
    OA = np.repeat((j < 64).astype(np.float32)[:, None], 128, 1)
    OB = np.repeat((j >= 64).astype(np.float32)[:, None], 128, 1)
    AD = (~((j[:, None] >= 64) & (j[None, :] < 64))).astype(np.float32)
    ID = np.eye(128, dtype=np.float32)
    ON = np.ones((128, 128), np.float32)
    masks = np.stack([V, U, OA, OB, AD, ID, ON], 1)
    inv = (500000.0 ** (-np.arange(0, 16, 2, dtype=np.float32) / 16)).astype(np.float32)
    ang = np.arange(S, dtype=np.float32)[:, None] * inv[None, :]
    rope = np.concatenate([np.cos(ang), np.sin(ang)], 1).astype(np.float32)
    return {"c_masks": np.ascontiguousarray(masks), "c_rope": rope}


W_SHAPES = {
    "norm_mix_w": (2, 1024), "w_in": (2, 1024, IN_COLS), "conv_w": (2, 4, 1536), "conv_b": (2, 1536),
    "dt_bias": (2, 16), "a_log": (2, 16), "d_skip": (2, 16), "ssd_norm_w": (2, 1024),
    "q_norm_w": (2, 64), "k_norm_w": (2, 64), "lambda_q1": (2, 64), "lambda_k1": (2, 64),
    "lambda_q2": (2, 64), "lambda_k2": (2, 64), "subln_w": (2, 128), "gate_b": (2, 2, 1024),
    "w_br_ssd": (2, 1024, 1024), "w_br_att": (2, 1024, 1024), "w_out": (2, 1024, 1024),
    "norm_ffn_w": (2, 1024), "ffn_w_gate": (1, 1024, DFF), "ffn_w_up": (1, 1024, DFF),
    "ffn_w_down": (1, DFF, 1024), "router_w": (1, 1024, 8), "moe_w_gate": (1, 8, 1024, DFF),
    "moe_w_up": (1, 8, 1024, DFF), "moe_w_down": (1, 8, DFF, 1024),
}


def build(S=4096, NL=2, debug=False, stop=None):
    NT = S // 128
    NB = S // 512
    nc = bass.Bass("TRN2", target_bir_lowering=False)
    fw = FW(nc)
    pe, act, dve, pool, sp = fw.pe, fw.act, fw.dve, fw.pool, fw.sp
    V_ = nc.vector
    A_ = nc.scalar
    G_ = nc.gpsimd
    T_ = nc.tensor

    def din(name, shape):
        return nc.dram_tensor(name, list(shape), F32, kind="ExternalInput").ap()
    I = {"x": din("x", (S, D))}
    for k, shp in W_SHAPES.items():
        I[k] = din(k, shp)
    c_masks = din("c_masks", (128, 7, 128))
    c_rope = din("c_rope", (S, 16))
    out = nc.dram_tensor("out", [S, D], F32, kind="ExternalOutput").ap()
    Bout = Buf("out"); Bout.dram = True
    Bin = Buf("in"); Bin.dram = True
    skind = "ExternalOutput" if debug else "Internal"

    def dscr(name, shape, dt):
        b = Buf(name); b.dram = True
        return nc.dram_tensor(name, list(shape), dt, kind=skind).ap(), b
    s_z, Bs_z = dscr("s_z", (S, D), BF16)
    s_q, Bs_q = dscr("s_q", (S, D), BF16)
    s_k, Bs_k = dscr("s_k", (S, D), BF16)
    s_v, Bs_v = dscr("s_v", (S, D), BF16)
    s_dt, Bs_dt = dscr("s_dt", (S, 16), F32)
    s_xbcT, Bs_xbcT = dscr("s_xbcT", (1536, S + 4), BF16)
    s_gT, Bs_gT = dscr("s_gT", (2048, S), BF16)
    s_qT, Bs_qT = dscr("s_qT", (8, 2, 128, S), BF16)
    s_kT, Bs_kT = dscr("s_kT", (8, 128, S), BF16)
    s_ysT, Bs_ysT = dscr("s_ysT", (D, S), BF16)
    s_yaT, Bs_yaT = dscr("s_yaT", (D, S), BF16)
    s_hT, Bs_hT = dscr("s_hT", (D, S), BF16)

    top = ExitStack()

    uniq = [0]

    def sbuf(st, name, shape, dt):
        uniq[0] += 1
        return st.enter_context(nc.sbuf_tensor(f"{name}_{uniq[0]}", list(shape), dt)), Buf(name)

    P = top.enter_context(nc.psum_tensor("P", [128, 8, 512], F32))
    BP = [Buf(f"P{i}") for i in range(8)]

    cm, Bcm = sbuf(top, "cm", (128, 7, 128), F32)
    cmb, Bcmb = sbuf(top, "cmb", (128, 7, 128), BF16)
    sp.dma(cm[:], c_masks[:, :, :], reads=[Bin], writes=[Bcm], own=Bcm)
    dve.op(lambda: V_.tensor_copy(out=cmb[:], in_=cm[:]), [Bcm], [Bcmb])
    Vf, Uf, OAf, OBf = cm[:, 0, :], cm[:, 1, :], cm[:, 2, :], cm[:, 3, :]
    ADb, IDb, ONb = cmb[:, 4, :], cmb[:, 5, :], cmb[:, 6, :]
    IDf = cm[:, 5, :]
    ONf = cm[:, 6, :]

    def rstd_from_ss(ss_ap, Bss, n):
        act.op(lambda: A_.activation(out=ss_ap, in_=ss_ap, func=AF.Sqrt, scale=1.0 / n, bias=EPS), [Bss], [Bss])
        dve.op(lambda: V_.reciprocal(out=ss_ap, in_=ss_ap), [Bss], [Bss])

    def norm_T(st, src, Bsrc, xT, BxT, tag, hook=None):
        NSL = 4
        xt = [sbuf(st, f"{tag}_x{i}", (128, D), F32) for i in range(NSL)]
        jks = [sbuf(st, f"{tag}_jk{i}", (128, D), BF16) for i in range(NSL)]
        ss = [sbuf(st, f"{tag}_ss{i}", (128, 1), F32) for i in range(NSL)]
        xn = [sbuf(st, f"{tag}_xn{i}", (128, D), BF16) for i in range(NSL)]

        def chain(t):
            i = t % NSL
            (x_, Bx), (ss_, Bss), (xn_, Bxn), (jk, Bjk) = xt[i], ss[i], xn[i], jks[i]
            sp.dma(x_[:], src[t * 128:(t + 1) * 128, :], reads=[Bsrc], writes=[Bx], own=Bx)
            yield
            act.op(lambda: A_.activation(out=jk[:], in_=x_[:], func=AF.Square, accum_out=ss_[:]), [Bx], [Bjk, Bss])
            yield
            act.op(lambda: A_.activation(out=ss_[:], in_=ss_[:], func=AF.Sqrt, scale=1.0 / D, bias=EPS), [Bss], [Bss])
            yield
            dve.op(lambda: V_.reciprocal(out=ss_[:], in_=ss_[:]), [Bss], [Bss])
            yield
            dve.op(lambda: V_.tensor_scalar(out=xn_[:], in0=x_[:], scalar1=ss_[:, 0:1], scalar2=None, op0=ALU.mult),
                   [Bx, Bss], [Bxn])
            yield
            if hook is not None:
                hook(t, x_, Bx, ss_, Bss)
                yield
            bank = i
            pv = P[:, bank, :].bitcast(BF16)
            for kc in range(8):
                pe.op(lambda: T_.transpose(out=pv[:, kc * 128:(kc + 1) * 128], in_=xn_[:, kc * 128:(kc + 1) * 128],
                                           identity=IDb), [Bxn, Bcmb], [BP[bank]])
            yield
            (dve if t % 2 else act).op(
                (lambda: V_.tensor_copy(out=xT[:, :, t * 128:(t + 1) * 128], in_=pv.rearrange("p (k t) -> p k t", k=8)))
                if t % 2 else
                (lambda: A_.copy(out=xT[:, :, t * 128:(t + 1) * 128], in_=pv.rearrange("p (k t) -> p k t", k=8))),
                [BP[bank]], [BxT[t]])
            yield

        for t0 in range(0, NT, NSL):
            live = [chain(t) for t in range(t0, min(NT, t0 + NSL))]
            while live:
                for g_ in list(live):
                    try:
                        next(g_)
                    except StopIteration:
                        live.remove(g_)
        return [b for _, b in xt]

    def load_w(st_bufs, slot, src_rows_ap, ncols, scale_ap, Bscale):
        (wf, Bwf), (wb, Bwb) = st_bufs[slot]
        sp.dma(wf[:, :, :ncols], src_rows_ap.rearrange("(kc p) c -> p kc c", p=128), reads=[Bin], writes=[Bwf], own=Bwf)
        if scale_ap is None:
            pool.op(lambda: G_.tensor_copy(out=wb[:, :, :ncols], in_=wf[:, :, :ncols]), [Bwf], [Bwb])
        else:
            pool.op(lambda: G_.tensor_tensor(out=wb[:, :, :ncols], in0=wf[:, :, :ncols],
                                             in1=scale_ap.unsqueeze(2).broadcast_to([128, 8, ncols]), op=ALU.mult),
                    [Bwf, Bscale], [Bwb])
        return wb, Bwb

    def col_load(st, name, src_1d, n):
        t, B = sbuf(st, name, (128, n), F32)
        sp.dma(t[:], src_1d.rearrange("(c p) -> p c", p=128), reads=[Bin], writes=[B], own=B, allow_slow_non_contiguous=True)
        return t, B

    def bc_load(st, name, src_1d, n):
        t, B = sbuf(st, name, (128, n), F32)
        sp.dma(t[:], src_1d.partition_broadcast(128), reads=[Bin], writes=[B], own=B)
        return t, B

    evac_rr = [0]

    def evac_copy(out_ap, in_ap, reads, writes):
        evac_rr[0] ^= 1
        if evac_rr[0]:
            act.op(lambda: A_.copy(out=out_ap, in_=in_ap), reads, writes)
        else:
            dve.op(lambda: V_.tensor_copy(out=out_ap, in_=in_ap), reads, writes)

    wrot = [0, 0]

    def cast_load(wfs, src3, nk, ncols, scale_ap, Bscale, dst_ap, Bdst):
        wf, Bwf = wfs[wrot[0] % len(wfs)]
        wrot[0] += 1
        sp.dma(wf[:, :nk, :ncols], src3, reads=[Bin], writes=[Bwf], own=Bwf)
        if scale_ap is None:
            pool.op(lambda: G_.tensor_copy(out=dst_ap, in_=wf[:, :nk, :ncols]), [Bwf], [Bdst])
        else:
            pool.op(lambda: G_.tensor_tensor(out=dst_ap, in0=wf[:, :nk, :ncols],
                                             in1=scale_ap.unsqueeze(2).broadcast_to([128, nk, ncols]), op=ALU.mult),
                    [Bwf, Bscale], [Bdst])

    for L in range(NL):
        lam_init = 0.8 - 0.6 * math.exp(-0.3 * L)
        src, Bsrc = (I["x"], Bin) if L == 0 else (out, Bout)

        with ExitStack() as st:
            xT, _ = sbuf(st, "xT", (128, 8, S), BF16)
            BxT = [Buf(f"xT{t}") for t in range(NT)]
            rel = norm_T(st, src, Bsrc, xT, BxT, "nA")
            nw, Bnw = col_load(st, "nw", I["norm_mix_w"][L], 8)
            gb, Bgb = sbuf(st, "gb", (128, 16), F32)
            sp.dma(gb[:], I["gate_b"][L].rearrange("b (c p) -> p (b c)", p=128), reads=[Bin], writes=[Bgb], own=Bgb,
                   allow_slow_non_contiguous=True)
            wbufs = [(sbuf(st, f"wf{i}", (128, 8, 512), F32), sbuf(st, f"wb{i}", (128, 8, 512), BF16)) for i in range(2)]
            stg = [sbuf(st, f"stg{i}", (128, 512), BF16) for i in range(4)]
            stgf = [sbuf(st, f"stgf{i}", (128, 16), F32) for i in range(2)]
            zt, Bzt = sbuf(st, "zt", (128, 4), BF16)
            pool.op(lambda: G_.memset(zt[:], 0.0), [], [Bzt])
            for c in range(12):
                sp.dma(s_xbcT[c * 128:(c + 1) * 128, 0:4], zt[:], reads=[Bzt], writes=[Bs_xbcT], own=Bzt)
            blocks = []
            for (o, dst, Bd) in ((O_Q, s_q, Bs_q), (O_K, s_k, Bs_k), (O_Z, s_z, Bs_z), (O_V, s_v, Bs_v)):
                for h in range(2):
                    blocks.append((o + h * 512, 512, "tok", dst, Bd, h * 512))
            blocks.append((O_DT, 16, "dt", s_dt, Bs_dt, 0))
            for h in range(3):
                blocks.append((O_XBC + h * 512, 512, "xbc", s_xbcT, Bs_xbcT, h * 512))
            for h in range(4):
                blocks.append((O_GS + h * 512, 512, "gate", s_gT, Bs_gT, h * 512))
            stateB = {"bi": -1, "g": 0}

            def phaseB_gen():
                pend = load_w(wbufs, 0, I["w_in"][L][:, blocks[0][0]:blocks[0][0] + blocks[0][1]], blocks[0][1], nw[:, :], Bnw)
                for bi, (c0, ncol, kind, dst, Bd, d0) in enumerate(blocks):
                    stateB["bi"] = bi
                    wb, Bwb = pend
                    if bi + 1 < len(blocks):
                        c0n, ncn = blocks[bi + 1][0], blocks[bi + 1][1]
                        pend = load_w(wbufs, (bi + 1) % 2, I["w_in"][L][:, c0n:c0n + ncn], ncn, nw[:, :], Bnw)
                    if kind in ("tok", "dt"):
                        for t in range(NT):
                            g = stateB["g"]
                            bank = g % 4
                            for kc in range(8):
                                pe.op(lambda: T_.matmul(P[:, bank, :ncol], lhsT=xT[:, kc, t * 128:(t + 1) * 128], rhs=wb[:, kc, :ncol],
                                                        start=(kc == 0), stop=(kc == 7)), [BxT[t], Bwb], [BP[bank]])
                            if kind == "tok":
                                s_, Bs_ = stg[g % 4]
                                act.op(lambda: A_.copy(out=s_[:], in_=P[:, bank, :]), [BP[bank]], [Bs_])
                                sp.dma(dst[t * 128:(t + 1) * 128, d0:d0 + 512], s_[:], reads=[Bs_], writes=[Bd], own=Bs_)
                            else:
                                s_, Bs_ = stgf[g % 2]
                                act.op(lambda: A_.copy(out=s_[:], in_=P[:, bank, :16]), [BP[bank]], [Bs_])
                                sp.dma(dst[t * 128:(t + 1) * 128, :], s_[:], reads=[Bs_], writes=[Bd], own=Bs_)
                            stateB["g"] += 1
                            yield
                    else:
                        for cc in range(4):
                            for tb in range(NB):
                                g = stateB["g"]
                                bank = g % 4
                                for kc in range(8):
                                    pe.op(lambda: T_.matmul(P[:, bank, :], lhsT=wb[:, kc, cc * 128:(cc + 1) * 128],
                                                            rhs=xT[:, kc, tb * 512:(tb + 1) * 512], start=(kc == 0), stop=(kc == 7)),
                                          BxT[tb * 4:tb * 4 + 4] + [Bwb], [BP[bank]])
                                s_, Bs_ = stg[g % 4]
                                r0 = d0 + cc * 128
                                if kind == "xbc":
                                    act.op(lambda: A_.copy(out=s_[:], in_=P[:, bank, :]), [BP[bank]], [Bs_])
                                    sp.dma(dst[r0:r0 + 128, 4 + tb * 512:4 + (tb + 1) * 512], s_[:], reads=[Bs_], writes=[Bd], own=Bs_)
                                else:
                                    gi = r0 // 128
                                    act.op(lambda: A_.activation(out=s_[:], in_=P[:, bank, :], func=AF.Sigmoid, bias=gb[:, gi:gi + 1]),
                                           [BP[bank], Bgb], [Bs_])
                                    sp.dma(dst[r0:r0 + 128, tb * 512:(tb + 1) * 512], s_[:], reads=[Bs_], writes=[Bd], own=Bs_)
                                stateB["g"] += 1
                                yield

            rope, Brope = sbuf(st, "rope", (128, NT, 16), F32)
            sp.dma(rope[:], c_rope.rearrange("(t p) c -> p t c", p=128), reads=[Bin], writes=[Brope], own=Brope)
            wq, Bwq = bc_load(st, "wq", I["q_norm_w"][L], 64)
            wk, Bwk = bc_load(st, "wk", I["k_norm_w"][L], 64)
            dve.op(lambda: V_.tensor_scalar(out=wq[:], in0=wq[:], scalar1=0.125, scalar2=None, op0=ALU.mult), [Bwq], [Bwq])
            d1b = []
            for qi in range(2):
                d = {}
                d["xr"] = [sbuf(st, f"xr{qi}_{i}", (128, 16, 64), BF16) for i in range(2)]
                d["sq"] = sbuf(st, f"sq{qi}", (128, 16, 64), F32)
                d["s16"] = sbuf(st, f"s16{qi}", (128, 16), F32)
                d["xnf"] = sbuf(st, f"xnf{qi}", (128, 16, 64), F32)
                d["xb"] = sbuf(st, f"xb{qi}", (128, 16, 64), BF16)
                d["tmp"] = [sbuf(st, f"rt{qi}_{i}", (128, 16, 8), F32) for i in range(4)]
                d1b.append(d)
            xTs = [sbuf(st, f"xTs{i}", (128, 8, 128), BF16) for i in range(2)]
            xTq = [[sbuf(st, f"xTq{i}_{j}", (128, 8, 128), BF16) for j in range(2)] for i in range(2)]
            for i in range(2):
                for j in range(2):
                    pool.op(lambda: G_.memset(xTq[i][j][0][:], 0.0), [], [xTq[i][j][1]])

            def d1_gen(qi):
                srcd, Bsrcd, wt, Bwt, dstT, BdstT = ((s_q, Bs_q, wq, Bwq, s_qT, Bs_qT), (s_k, Bs_k, wk, Bwk, s_kT, Bs_kT))[qi]
                d = d1b[qi]
                (sq, Bsq), (s16, Bs16), (xnf, Bxnf), (xb_, Bxb_), tmp = d["sq"], d["s16"], d["xnf"], d["xb"], d["tmp"]
                for t in range(NT):
                    x_, Bx_ = d["xr"][t % 2]
                    sp.dma(x_[:].rearrange("p a b -> p (a b)"), srcd[t * 128:(t + 1) * 128, :], reads=[Bsrcd], writes=[Bx_], own=Bx_)
                    yield
                    dve.op(lambda: V_.tensor_tensor(out=sq[:], in0=x_[:], in1=x_[:], op=ALU.mult), [Bx_], [Bsq])
                    yield
                    dve.op(lambda: V_.tensor_reduce(out=s16[:], in_=sq[:], axis=AX.X, op=ALU.add), [Bsq], [Bs16])
                    yield
                    act.op(lambda: A_.activation(out=s16[:], in_=s16[:], func=AF.Sqrt, scale=1.0 / 64, bias=EPS), [Bs16], [Bs16])
                    yield
                    dve.op(lambda: V_.reciprocal(out=s16[:], in_=s16[:]), [Bs16], [Bs16])
                    yield
                    dve.op(lambda: V_.tensor_tensor(out=xnf[:], in0=x_[:], in1=s16[:].unsqueeze(2).broadcast_to([128, 16, 64]),
                                                    op=ALU.mult), [Bx_, Bs16], [Bxnf])
                    yield
                    pool.op(lambda: G_.tensor_tensor(out=xnf[:], in0=xnf[:], in1=wt[:].unsqueeze(1).broadcast_to([128, 16, 64]),
                                                     op=ALU.mult), [Bxnf, Bwt], [Bxnf])
                    yield
                    dve.op(lambda: V_.tensor_copy(out=xb_[:], in_=xnf[:]), [Bxnf], [Bxb_])
                    yield
                    cs = rope[:, t, 0:8].unsqueeze(1).broadcast_to([128, 16, 8])
                    sn = rope[:, t, 8:16].unsqueeze(1).broadcast_to([128, 16, 8])
                    r1, r2 = xnf[:, :, 0:8], xnf[:, :, 8:16]
                    for i, (a_, b_) in enumerate(((r1, cs), (r2, sn), (r2, cs), (r1, sn))):
                        dve.op(lambda: V_.tensor_tensor(out=tmp[i][0][:], in0=a_, in1=b_, op=ALU.mult), [Bxnf, Brope], [tmp[i][1]])
                        yield
                    dve.op(lambda: V_.tensor_tensor(out=xb_[:, :, 0:8], in0=tmp[0][0][:], in1=tmp[1][0][:], op=ALU.subtract),
                           [tmp[0][1], tmp[1][1]], [Bxb_])
                    yield
                    dve.op(lambda: V_.tensor_tensor(out=xb_[:, :, 8:16], in0=tmp[2][0][:], in1=tmp[3][0][:], op=ALU.add),
                           [tmp[2][1], tmp[3][1]], [Bxb_])
                    for _ in range(8):
                        yield
                    bank = 4 + 2 * qi + t % 2
                    pv = P[:, bank, :].bitcast(BF16)
                    for h in range(8):
                        pe.op(lambda: T_.transpose(out=pv[:, h * 128:(h + 1) * 128],
                                                   in_=xb_[:, 2 * h:2 * h + 2, :].rearrange("p a b -> p (a b)"), identity=IDb),
                              [Bxb_, Bcmb], [BP[bank]])
                    yield
                    pv3 = pv.rearrange("p (k t) -> p k t", k=8)
                    if qi == 0:
                        for j in range(2):
                            xq_, Bxq_ = xTq[t % 2][j]
                            evac_copy(xq_[j * 64:(j + 1) * 64], pv3[j * 64:(j + 1) * 64], [BP[bank]], [Bxq_])
                            sp.dma(s_qT[:, j].rearrange("h p s -> p h s")[:, :, t * 128:(t + 1) * 128], xq_[:], reads=[Bxq_],
                                   writes=[BdstT], own=Bxq_)
                    else:
                        xT_, BxT_ = xTs[t % 2]
                        evac_copy(xT_[:], pv3, [BP[bank]], [BxT_])
                        sp.dma(dstT.rearrange("h p s -> p h s")[:, :, t * 128:(t + 1) * 128], xT_[:], reads=[BxT_], writes=[BdstT], own=BxT_)
                    yield

            def zipper_w(pairs):
                live = [[g_, w_] for g_, w_ in pairs]
                while live:
                    for it in list(live):
                        for _ in range(it[1]):
                            try:
                                next(it[0])
                            except StopIteration:
                                live.remove(it)
                                break

            gB = phaseB_gen()
            while stateB["bi"] < 4:
                next(gB)
            zipper_w([(gB, 1), (d1_gen(0), 2), (d1_gen(1), 2)])
            fw.barrier()
            fw.release([Brope, Bwq, Bwk] + [b for _, b in d1b[0]["xr"] + d1b[1]["xr"] + xTs + xTq[0] + xTq[1]])
            fw.release(rel + [Bnw, Bgb, Bzt] + [b for (_, b) in stg] + [b for (_, b) in stgf] + [w[0][1] for w in wbufs])
        if stop == "B":
            break

        with ExitStack() as st:
            cw, Bcw = sbuf(st, "cw", (128, 4, 12), F32)
            for k in range(4):
                sp.dma(cw[:, k, :], I["conv_w"][L][k].rearrange("(c p) -> p c", p=128), reads=[Bin], writes=[Bcw], own=Bcw,
                       allow_slow_non_contiguous=True)
            cbc, Bcbc = col_load(st, "cbc", I["conv_b"][L], 12)
            cbrf, Bcbrf = sbuf(st, "cbrf", (1, 1280), F32)
            sp.dma(cbrf[:], I["conv_b"][L][0:1280].unsqueeze(0), reads=[Bin], writes=[Bcbrf], own=Bcbrf)
            cbr, Bcbr = sbuf(st, "cbr", (1, 1280), BF16)
            dve.op(lambda: V_.tensor_copy(out=cbr[:], in_=cbrf[:]), [Bcbrf], [Bcbr])
            diag, Bdiag = sbuf(st, "diag", (128, 12, 4, 128), BF16)
            for c in range(12):
                pool.op(lambda: G_.tensor_tensor(out=diag[:, c, :, :], in0=IDf.unsqueeze(1).broadcast_to([128, 4, 128]),
                                                 in1=cw[:, :, c].unsqueeze(2).broadcast_to([128, 4, 128]), op=ALU.mult),
                        [Bcm, Bcw], [Bdiag])
            dtb, Bdtb = bc_load(st, "dtb", I["dt_bias"][L], 16)
            aneg, Baneg = bc_load(st, "aneg", I["a_log"][L], 16)
            dsk, Bdsk = bc_load(st, "dsk", I["d_skip"][L], 16)
            act.op(lambda: A_.activation(out=aneg[:], in_=aneg[:], func=AF.Exp), [Baneg], [Baneg])
            dve.op(lambda: V_.tensor_scalar(out=aneg[:], in0=aneg[:], scalar1=-1.0, scalar2=None, op0=ALU.mult), [Baneg], [Baneg])
            hst, Bhst = sbuf(st, "hst", (128, 16, 64), F32)
            hbf = [sbuf(st, f"hbf{i}", (128, 1024), BF16) for i in range(2)]
            dve.op(lambda: V_.memset(hst[:], 0.0), [], [Bhst])
            dve.op(lambda: V_.memset(hbf[0][0][:], 0.0), [], [hbf[0][1]])
            Uw = [sbuf(st, f"Uw{i}", (128, 12, 132), BF16) for i in range(2)]
            dtr = [sbuf(st, f"dtr{i}", (128, 16), F32) for i in range(2)]
            ztl = [sbuf(st, f"ztl{i}", (128, 1024), BF16) for i in range(3)]
            xs2 = [sbuf(st, f"xs{i}", (128, 16, 64), BF16) for i in range(3)]
            Btk2 = [sbuf(st, f"Btk{i}", (128, 256), BF16) for i in range(2)]
            BCT2 = [sbuf(st, f"BCT{i}", (128, 4, 128), BF16) for i in range(2)]
            xdte2 = [sbuf(st, f"xdte{i}", (128, 16, 64), BF16) for i in range(2)]
            sm2 = [sbuf(st, f"sm{i}", (128, 64), F32) for i in range(2)]
            ydg2 = [sbuf(st, f"ydg{i}", (128, 16, 64), F32) for i in range(3)]
            t1s = [sbuf(st, f"t1s{i}", (128, 16, 64), F32) for i in range(2)]
            dt_, Bdt_ = sbuf(st, "dt_", (128, 16), F32)
            adt, Badt = sbuf(st, "adt", (128, 16), F32)
            R_, BR_ = sbuf(st, "R_", (128, 16, 128), F32)
            Es, BEs = sbuf(st, "Es", (128, 16, 128), F32)
            cbm, Bcbm = sbuf(st, "cbm", (128, 2, 128), F32)
            MT, BMT = sbuf(st, "MT", (128, 16, 128), BF16)
            dtd, Bdtd = sbuf(st, "dtd", (128, 16), F32)
            xdt, Bxdt = sbuf(st, "xdt", (128, 16, 64), BF16)
            t1, Bt1 = sbuf(st, "t1", (128, 16, 64), F32)
            t2, Bt2 = sbuf(st, "t2", (128, 16, 64), F32)
            sz, Bsz = sbuf(st, "sz", (128, 1024), F32)
            jk, Bjk = sbuf(st, "jkC", (128, 512), F32)
            ss2, Bss2 = sbuf(st, "ss2", (128, 2), F32)
            yn, Byn = sbuf(st, "yn", (128, 1024), BF16)
            yTs = [sbuf(st, f"yTs{i}", (128, 8, 128), BF16) for i in range(2)]
            xbcT_v = s_xbcT.rearrange("(c p) s -> p c s", p=128)
            ysT_v = s_ysT.rearrange("(k p) s -> p k s", p=128)

            NS = NT * 16
            dtA, BdtA = sbuf(st, "dtA", (128, NT, 16), F32)
            adtA, BadtA = sbuf(st, "adtA", (128, NT, 16), F32)
            smA, BsmA = sbuf(st, "smA", (128, 4, NS), F32)
            dtdA, BdtdA = sbuf(st, "dtdA", (128, NT, 16), F32)
            sp.dma(dtA[:], s_dt.rearrange("(t p) h -> p t h", p=128), reads=[Bs_dt], writes=[BdtA], own=BdtA)
            dve.op(lambda: V_.tensor_tensor(out=dtA[:], in0=dtA[:], in1=dtb[:].unsqueeze(1).broadcast_to([128, NT, 16]), op=ALU.add),
                   [BdtA, Bdtb], [BdtA])
            act.op(lambda: A_.activation(out=dtA[:], in_=dtA[:], func=AF.Exp), [BdtA], [BdtA])
            act.op(lambda: A_.activation(out=dtA[:], in_=dtA[:], func=AF.Ln, bias=1.0), [BdtA], [BdtA])
            dve.op(lambda: V_.tensor_tensor(out=adtA[:], in0=dtA[:], in1=aneg[:].unsqueeze(1).broadcast_to([128, NT, 16]), op=ALU.mult),
                   [BdtA, Baneg], [BadtA])
            for i, m_ in enumerate((Vf, Uf, OAf, OBf)):
                pe.op(lambda: T_.matmul(P[:, i, 0:NS], lhsT=m_, rhs=adtA[:].rearrange("p t h -> p (t h)"), start=True, stop=True),
                      [Bcm, BadtA], [BP[i]])
            act.op(lambda: A_.activation(out=smA[:], in_=P[:, 0:4, 0:NS], func=AF.Exp), [BP[0], BP[1], BP[2], BP[3]], [BsmA])
            dve.op(lambda: V_.tensor_tensor(out=dtdA[:].rearrange("p t h -> p (t h)"), in0=dtA[:].rearrange("p t h -> p (t h)"),
                                            in1=smA[:, 1, :], op=ALU.mult), [BdtA, BsmA], [BdtdA])

            def ssd_stage1(t):
                i2 = t % 2
                i3 = t % 3
                b0 = 4 * i2
                (U_, BU_), (dtr_, Bdtr_), (zt_, Bzt_) = Uw[i2], dtr[i2], ztl[i3]
                (xs, Bxs), (Btk, BBtk), (BCT, BBCT) = xs2[i3], Btk2[i2], BCT2[i2]
                (xdte, Bxdte), (sm, Bsm), (ydg, Bydg) = xdte2[i2], sm2[i2], ydg2[i3]
                sp.dma(U_[:, :, 0:131], xbcT_v[:, :, t * 128 + 1:t * 128 + 132], reads=[Bs_xbcT], writes=[BU_], own=BU_)
                sp.dma(zt_[:], s_z[t * 128:(t + 1) * 128, :], reads=[Bs_z], writes=[Bzt_], own=Bzt_)
                yield
                for c in range(10):
                    bank, col = (b0 + c // 4, (c % 4) * 128) if c < 8 else (b0 + 3, (c - 8) * 128)
                    o_ = P[:, bank, col:col + 128]
                    for k in range(4):
                        pe.op(lambda: T_.matmul(o_, lhsT=U_[:, c, k:k + 128], rhs=diag[:, c, k, :], start=(k == 0), stop=False),
                              [BU_, Bdiag], [BP[bank]])
                    pe.op(lambda: T_.matmul(o_, lhsT=ONb[0:1, :], rhs=cbr[0:1, c * 128:(c + 1) * 128], start=False, stop=True),
                          [Bcmb, Bcbr], [BP[bank]])
                    yield
                for i, c in enumerate(range(8, 12)):
                    for k in range(4):
                        pe.op(lambda: T_.matmul(P[:, b0 + 2, i * 128:(i + 1) * 128], lhsT=diag[:, c, k, :], rhs=U_[:, c, k:k + 128],
                                                start=(k == 0), stop=(k == 3)), [BU_, Bdiag], [BP[b0 + 2]])
                    yield
                act.op(lambda: A_.activation(out=xs[:].rearrange("p (b h) d -> p b (h d)", b=2), in_=P[:, b0:b0 + 2, :], func=AF.Silu),
                       [BP[b0], BP[b0 + 1]], [Bxs])
                act.op(lambda: A_.activation(out=Btk[:], in_=P[:, b0 + 3, 0:256], func=AF.Silu), [BP[b0 + 3]], [BBtk])
                yield
                for i in range(4):
                    act.op(lambda: A_.activation(out=BCT[:, i, :], in_=P[:, b0 + 2, i * 128:(i + 1) * 128], func=AF.Silu,
                                                 bias=cbc[:, 8 + i:9 + i]), [BP[b0 + 2], Bcbc], [BBCT])
                yield
                for g in range(2):
                    pe.op(lambda: T_.matmul(P[:, b0 + 3, 256 + g * 128:256 + (g + 1) * 128], lhsT=BCT[:, g, :], rhs=BCT[:, 2 + g, :],
                                            start=True, stop=True), [BBCT], [BP[b0 + 3]])
                dve.op(lambda: V_.tensor_tensor(out=cbm[:], in0=P[:, b0 + 3, 256:512].rearrange("p (g l) -> p g l", g=2),
                                                in1=Vf.unsqueeze(1).broadcast_to([128, 2, 128]), op=ALU.mult),
                       [BP[b0 + 3], Bcm], [Bcbm])
                yield
                dve.op(lambda: V_.tensor_tensor(out=xdt[:], in0=xs[:], in1=dtA[:, t, :].unsqueeze(2).broadcast_to([128, 16, 64]),
                                                op=ALU.mult), [Bxs, BdtA], [Bxdt])
                yield
                pool.op(lambda: G_.tensor_tensor(out=xdte[:], in0=xs[:], in1=dtdA[:, t, :].unsqueeze(2).broadcast_to([128, 16, 64]),
                                                 op=ALU.mult), [Bxs, BdtdA], [Bxdte])
                dve.op(lambda: V_.tensor_tensor(out=R_[:], in0=adtA[:, t, :].unsqueeze(2).broadcast_to([128, 16, 128]),
                                                in1=Vf.unsqueeze(1).broadcast_to([128, 16, 128]), op=ALU.mult),
                       [BadtA, Bcm], [BR_])
                yield
                for hq in range(4):
                    pe.op(lambda: T_.matmul(P[:, b0 + hq, :], lhsT=Uf, rhs=R_[:, hq * 4:(hq + 1) * 4, :].rearrange("p a b -> p (a b)"),
                                            start=True, stop=True), [Bcm, BR_], [BP[b0 + hq]])
                    yield
                act.op(lambda: A_.activation(out=Es[:].rearrange("p (a b) l -> p a (b l)", a=4), in_=P[:, b0:b0 + 4, :], func=AF.Exp),
                       [BP[b0], BP[b0 + 1], BP[b0 + 2], BP[b0 + 3]], [BEs])
                yield
                dve.op(lambda: V_.tensor_tensor(out=MT[:].rearrange("p (g e) l -> p g e l", g=2),
                                                in0=Es[:].rearrange("p (g e) l -> p g e l", g=2),
                                                in1=cbm[:].unsqueeze(2).broadcast_to([128, 2, 8, 128]), op=ALU.mult),
                       [BEs, Bcbm], [BMT])
                yield
                for h in range(16):
                    pe.op(lambda: T_.matmul(P[:, b0 + h // 8, (h % 8) * 64:(h % 8 + 1) * 64], lhsT=MT[:, h, :], rhs=xdt[:, h, :],
                                            start=True, stop=True), [BMT, Bxdt], [BP[b0 + h // 8]])
                    if h % 4 == 3:
                        yield
                evac_copy(ydg[:].rearrange("p (b h) d -> p b (h d)", b=2), P[:, b0:b0 + 2, :], [BP[b0], BP[b0 + 1]], [Bydg])
                yield

            def ssd_stage2(t):
                i2 = t % 2
                b0 = 4 * i2
                (Btk, BBtk), (BCT, BBCT), (xdte, Bxdte) = Btk2[i2], BCT2[i2], xdte2[i2]
                t1, Bt1 = t1s[i2]
                for ch in range(2):
                    r0 = ch * 64
                    (hb_in, Bhb_in), (hb_out, Bhb_out) = hbf[ch], hbf[1 - ch]
                    for g in range(2):
                        pe.op(lambda: T_.matmul(P[r0:r0 + 64, b0 + g, :], lhsT=BCT[:, 2 + g, r0:r0 + 64],
                                                rhs=hb_in[:, g * 512:(g + 1) * 512], start=True, stop=True),
                              [BBCT, Bhb_in], [BP[b0 + g]])
                    for g in range(2):
                        pe.op(lambda: T_.matmul(P[:, b0 + 2 + g, :], lhsT=Btk[r0:r0 + 64, g * 128:(g + 1) * 128],
                                                rhs=xdte[r0:r0 + 64, g * 8:(g + 1) * 8, :].rearrange("p a b -> p (a b)"),
                                                start=True, stop=True), [BBtk, Bxdte], [BP[b0 + 2 + g]])
                    yield
                    cd = smA[:, 2 + ch, t * 16:(t + 1) * 16]
                    dve.op(lambda: V_.tensor_tensor(out=hst[:], in0=hst[:], in1=cd.unsqueeze(2).broadcast_to([128, 16, 64]),
                                                    op=ALU.mult), [Bhst, BsmA], [Bhst])
                    yield
                    dve.op(lambda: V_.tensor_tensor(out=hst[:].rearrange("p (b h) d -> p b (h d)", b=2),
                                                    in0=hst[:].rearrange("p (b h) d -> p b (h d)", b=2), in1=P[:, b0 + 2:b0 + 4, :],
                                                    op=ALU.add), [Bhst, BP[b0 + 2], BP[b0 + 3]], [Bhst])
                    yield
                    act.op(lambda: A_.copy(out=hb_out[:], in_=hst[:].rearrange("p h d -> p (h d)")), [Bhst], [Bhb_out])
                    yield
                dve.op(lambda: V_.tensor_tensor(out=t1[:], in0=P[:, b0:b0 + 2, :].rearrange("p b (h d) -> p (b h) d", d=64),
                                                in1=smA[:, 0, t * 16:(t + 1) * 16].unsqueeze(2).broadcast_to([128, 16, 64]), op=ALU.mult),
                       [BP[b0], BP[b0 + 1], BsmA], [Bt1])
                yield

            def ssd_stage3(t):
                i2 = t % 2
                i3 = t % 3
                b0 = 4 * i2
                (zt_, Bzt_), (xs, Bxs), (ydg, Bydg) = ztl[i3], xs2[i3], ydg2[i3]
                t1, Bt1 = t1s[i2]
                pool.op(lambda: G_.tensor_tensor(out=t2[:], in0=xs[:], in1=dsk[:].unsqueeze(2).broadcast_to([128, 16, 64]),
                                                 op=ALU.mult), [Bxs, Bdsk], [Bt2])
                yield
                dve.op(lambda: V_.tensor_tensor(out=t1[:], in0=t1[:], in1=ydg[:], op=ALU.add), [Bt1, Bydg], [Bt1])
                act.op(lambda: A_.activation(out=sz[:], in_=zt_[:], func=AF.Silu), [Bzt_], [Bsz])
                yield
                dve.op(lambda: V_.tensor_tensor(out=t1[:], in0=t1[:], in1=t2[:], op=ALU.add), [Bt1, Bt2], [Bt1])
                yield
                dve.op(lambda: V_.tensor_tensor(out=t1[:].rearrange("p h d -> p (h d)"), in0=t1[:].rearrange("p h d -> p (h d)"),
                                                in1=sz[:], op=ALU.mult), [Bt1, Bsz], [Bt1])
                yield
                t1f = t1[:].rearrange("p h d -> p (h d)")
                for g in range(2):
                    act.op(lambda: A_.activation(out=jk[:], in_=t1f[:, g * 512:(g + 1) * 512], func=AF.Square,
                                                 accum_out=ss2[:, g:g + 1]), [Bt1], [Bjk, Bss2])
                    yield
                act.op(lambda: A_.activation(out=ss2[:], in_=ss2[:], func=AF.Sqrt, scale=1.0 / 512, bias=EPS), [Bss2], [Bss2])
                yield
                dve.op(lambda: V_.reciprocal(out=ss2[:], in_=ss2[:]), [Bss2], [Bss2])
                yield
                for g in range(2):
                    pool.op(lambda: G_.tensor_scalar(out=yn[:, g * 512:(g + 1) * 512], in0=t1f[:, g * 512:(g + 1) * 512],
                                                     scalar1=ss2[:, g:g + 1], scalar2=None, op0=ALU.mult), [Bt1, Bss2], [Byn])
                yield

            def ssd_stage4(t):
                bank = 4 * ((t + 1) % 2) + 2
                pv = P[:, bank, :].bitcast(BF16)
                for kc in range(8):
                    pe.op(lambda: T_.transpose(out=pv[:, kc * 128:(kc + 1) * 128], in_=yn[:, kc * 128:(kc + 1) * 128],
                                               identity=IDb), [Byn, Bcmb], [BP[bank]])
                yT_, ByT_ = yTs[t % 2]
                evac_copy(yT_[:], pv.rearrange("p (k t) -> p k t", k=8), [BP[bank]], [ByT_])
                sp.dma(ysT_v[:, :, t * 128:(t + 1) * 128], yT_[:], reads=[ByT_], writes=[Bs_ysT], own=ByT_)

            def zipper(*gens):
                live = [g_ for g_ in gens if g_ is not None]
                while live:
                    for g_ in list(live):
                        try:
                            next(g_)
                        except StopIteration:
                            live.remove(g_)

            zipper(ssd_stage1(0))
            for t in range(NT + 1):
                zipper(ssd_stage1(t + 1) if t + 1 < NT else None,
                       ssd_stage2(t) if t < NT else None,
                       ssd_stage3(t - 1) if t >= 1 else None)
                if t >= 1:
                    ssd_stage4(t - 1)
            fw.barrier()
            fw.release([Bcw, Bcbc, Bcbrf, Bdtb, Baneg, Bdsk, BdtA] + [b for _, b in Uw + dtr + ztl + yTs])
        if stop == "C":
            break

        stE = ExitStack()
        wfsE = [sbuf(stE, f"wfE{i}", (128, 8, 512), F32) for i in range(2)]
        snw, Bsnw = col_load(stE, "snw", I["ssd_norm_w"][L], 8)
        sw1, Bsw1 = col_load(stE, "sw1", I["subln_w"][L], 1)
        sw8, Bsw8 = sbuf(stE, "sw8", (128, 8), F32)
        dve.op(lambda: V_.tensor_scalar(out=sw8[:], in0=sw1[:, 0:1].broadcast_to([128, 8]), scalar1=1.0 - lam_init, scalar2=None,
                                        op0=ALU.mult), [Bsw1], [Bsw8])
        Wr = [sbuf(stE, f"Wr{i}", (128, 8, 1024), BF16) for i in range(3)]

        def prefetch_E():
            for wi, (nm, sc, Bsc) in enumerate((("w_br_ssd", snw, Bsnw), ("w_br_att", sw8, Bsw8), ("w_out", None, None))):
                for hf in range(2):
                    cast_load(wfsE, I[nm][L][:, hf * 512:(hf + 1) * 512].rearrange("(k p) c -> p k c", p=128), 8, 512,
                              None if sc is None else sc[:, :], Bsc, Wr[wi][0][:, :, hf * 512:(hf + 1) * 512], Wr[wi][1])

        with ExitStack() as st:
            lv = [bc_load(st, f"lv{i}", I[n][L], 64) for i, n in enumerate(("lambda_q1", "lambda_k1", "lambda_q2", "lambda_k2"))]
            lp, Blp = sbuf(st, "lp", (128, 64), F32)
            le, Ble = sbuf(st, "le", (128, 2), F32)
            nlam, Bnlam = sbuf(st, "nlam", (128, 1), F32)
            for i in range(2):
                dve.op(lambda: V_.tensor_tensor(out=lp[:], in0=lv[2 * i][0][:], in1=lv[2 * i + 1][0][:], op=ALU.mult),
                       [lv[2 * i][1], lv[2 * i + 1][1]], [Blp])
                dve.op(lambda: V_.tensor_reduce(out=le[:, i:i + 1], in_=lp[:], axis=AX.X, op=ALU.add), [Blp], [Ble])
            act.op(lambda: A_.activation(out=le[:], in_=le[:], func=AF.Exp), [Ble], [Ble])
            dve.op(lambda: V_.tensor_tensor(out=nlam[:], in0=le[:, 1:2], in1=le[:, 0:1], op=ALU.subtract), [Ble], [Bnlam])
            dve.op(lambda: V_.tensor_scalar(out=nlam[:], in0=nlam[:], scalar1=-lam_init, scalar2=None, op0=ALU.add), [Bnlam], [Bnlam])
            QT = [sbuf(st, f"QT{i}", (128, 2, S), BF16) for i in range(2)]
            KT = [sbuf(st, f"KT{i}", (128, S), BF16) for i in range(2)]
            Vh = [sbuf(st, f"Vh{i}", (128, NT, 130), BF16) for i in range(2)]
            PT = [sbuf(st, f"PT{i}", (128, 512), BF16) for i in range(4)]
            accs = [sbuf(st, f"accs{i}", (128, 4, 386), F32) for i in range(2)]
            rs, Brs = sbuf(st, "rs", (128, 4, 2), F32)
            c1, Bc1 = sbuf(st, "c1", (128, 4), F32)
            t0, Bt0 = sbuf(st, "t0", (128, 4, 128), F32)
            t1, Bt1 = sbuf(st, "t1D", (128, 4, 128), F32)
            ssd_, Bssd = sbuf(st, "ssd", (128, 4), F32)
            yb, Byb = sbuf(st, "yb", (128, 4, 128), BF16)
            yaTs = [sbuf(st, f"yaTs{i}", (128, 512), BF16) for i in range(2)]
            for i in range(2):
                pool.op(lambda: G_.memset(Vh[i][0][:, :, 128:130], 1.0), [], [Vh[i][1]])
            LOOK = 3
            rot = {"gs": 0, "ge": 0, "pt": 0}

            def emit_scores(stp):
                h, qb, kt, j, QT_, BQT_, KT_, BKT_, Vh_, BVh_ = stp["a"]
                i = kt - 4 * qb
                qlo = 0 if i < 0 else i * 128
                sb = rot["gs"] % 4
                rot["gs"] += 1
                PT_, BPT_ = PT[rot["pt"] % 4]
                rot["pt"] += 1
                stp["pt"] = (PT_, BPT_)
                pe.op(lambda: T_.matmul(P[:, sb, qlo:512], lhsT=KT_[:, kt * 128:(kt + 1) * 128],
                                        rhs=QT_[:, j, qb * 512 + qlo:(qb + 1) * 512], start=True, stop=True),
                      [BKT_, BQT_], [BP[sb]])
                act.op(lambda: A_.activation(out=PT_[:, qlo:512], in_=P[:, sb, qlo:512], func=AF.Exp), [BP[sb]], [BPT_])
                if i >= 0:
                    pool.op(lambda: G_.tensor_tensor(out=PT_[:, qlo:qlo + 128], in0=PT_[:, qlo:qlo + 128], in1=ADb, op=ALU.mult),
                            [BPT_, Bcmb], [BPT_])

            def emit_av(stp):
                h, qb, kt, j, QT_, BQT_, KT_, BKT_, Vh_, BVh_ = stp["a"]
                i = kt - 4 * qb
                PT_, BPT_ = stp["pt"]
                for s_ in range(max(i, 0), 4):
                    first = (kt == 0 and j == 0)
                    last = (kt == 4 * qb + s_) and j == 1
                    pe.op(lambda: T_.matmul(P[:, 4 + s_, j * 256:j * 256 + 129], lhsT=PT_[:, s_ * 128:(s_ + 1) * 128],
                                            rhs=Vh_[:, kt, 0:129], start=first, stop=last, skip_group_check=True),
                          [BPT_, BVh_], [BP[4 + s_]])
                if kt == 4 * qb + 3 and j == 1:
                    emit_epi(h, qb)

            def emit_epi(h, qb):
                ac, Bac = accs[rot["ge"] % 2]
                ya_, Bya_ = yaTs[rot["ge"] % 2]
                rot["ge"] += 1
                dve.op(lambda: V_.tensor_copy(out=ac[:, :, 0:385], in_=P[:, 4:8, 0:385]), [BP[4], BP[5], BP[6], BP[7]], [Bac])
                dve.op(lambda: V_.reciprocal(out=rs[:], in_=ac[:, :, 128:385:256]), [Bac], [Brs])
                dve.op(lambda: V_.tensor_tensor(out=c1[:], in0=rs[:, :, 1], in1=nlam[:, 0:1].broadcast_to([128, 4]), op=ALU.mult),
                       [Brs, Bnlam], [Bc1])
                dve.op(lambda: V_.tensor_tensor(out=t0[:], in0=ac[:, :, 0:128], in1=rs[:, :, 0:1].broadcast_to([128, 4, 128]), op=ALU.mult),
                       [Bac, Brs], [Bt0])
                dve.op(lambda: V_.tensor_tensor(out=t1[:], in0=ac[:, :, 256:384], in1=c1[:].unsqueeze(2).broadcast_to([128, 4, 128]),
                                                op=ALU.mult), [Bac, Bc1], [Bt1])
                dve.op(lambda: V_.tensor_tensor(out=t0[:], in0=t0[:], in1=t1[:], op=ALU.add), [Bt0, Bt1], [Bt0])
                dve.op(lambda: V_.tensor_tensor(out=t1[:], in0=t0[:], in1=t0[:], op=ALU.mult), [Bt0], [Bt1])
                dve.op(lambda: V_.tensor_reduce(out=ssd_[:], in_=t1[:], axis=AX.X, op=ALU.add), [Bt1], [Bssd])
                rstd_from_ss(ssd_[:], Bssd, 128)
                dve.op(lambda: V_.tensor_tensor(out=yb[:], in0=t0[:], in1=ssd_[:].unsqueeze(2).broadcast_to([128, 4, 128]), op=ALU.mult),
                       [Bt0, Bssd], [Byb])
                eb = rot["gs"] % 4
                rot["gs"] += 1
                pv = P[:, eb, :].bitcast(BF16)
                for s_ in range(4):
                    pe.op(lambda: T_.transpose(out=pv[:, s_ * 128:(s_ + 1) * 128], in_=yb[:, s_, :], identity=IDb), [Byb, Bcmb], [BP[eb]])
                dve.op(lambda: V_.tensor_copy(out=ya_[:], in_=pv[:, 0:512]), [BP[eb]], [Bya_])
                sp.dma(s_yaT[h * 128:(h + 1) * 128, qb * 512:(qb + 1) * 512], ya_[:], reads=[Bya_], writes=[Bs_yaT], own=Bya_)

            def load_head(h):
                (QT_, BQT_), (KT_, BKT_), (Vh_, BVh_) = QT[h % 2], KT[h % 2], Vh[h % 2]
                sp.dma(QT_[:], s_qT[h].rearrange("j p s -> p j s"), reads=[Bs_qT], writes=[BQT_], own=BQT_)
                sp.dma(KT_[:], s_kT[h], reads=[Bs_kT], writes=[BKT_], own=BKT_)
                sp.dma(Vh_[:, :, 0:128], s_v.rearrange("(t p) c -> p t c", p=128)[:, :, h * 128:(h + 1) * 128],
                       reads=[Bs_v], writes=[BVh_], own=BVh_)

            steps = []
            for h in range(8):
                (QT_, BQT_), (KT_, BKT_), (Vh_, BVh_) = QT[h % 2], KT[h % 2], Vh[h % 2]
                for qb in range(NB):
                    for kt in range(4 * qb + 4):
                        for j in range(2):
                            steps.append({"a": (h, qb, kt, j, QT_, BQT_, KT_, BKT_, Vh_, BVh_), "load": (qb == 0 and kt == 0 and j == 0)})
            for n in range(len(steps) + LOOK):
                if n < len(steps):
                    stp = steps[n]
                    if stp["load"] and stp["a"][0] == 0:
                        for h in (0, 1):
                            load_head(h)
                        prefetch_E()
                    emit_scores(stp)
                if n - LOOK >= 0:
                    sm_ = steps[n - LOOK]
                    if sm_["load"] and 1 <= sm_["a"][0] <= 6:
                        load_head(sm_["a"][0] + 1)
                    emit_av(sm_)
            fw.barrier()
            fw.release([b for _, b in lv + QT + KT + Vh + yaTs])
        if stop == "D":
            stE.close()
            break

        with ExitStack() as st:
            (Wbs, BWbs), (Wba, BWba), (Wo, BWo) = Wr
            ysb = [sbuf(st, f"ysb{i}", (128, 8, 512), BF16) for i in range(2)]
            yab = [sbuf(st, f"yab{i}", (128, 8, 512), BF16) for i in range(2)]
            gtb = [sbuf(st, f"gtb{i}", (128, 16, 512), BF16) for i in range(2)]
            m1, Bm1 = sbuf(st, "m1", (128, 512), F32)
            m2, Bm2 = sbuf(st, "m2", (128, 512), F32)
            mT, BmT = sbuf(st, "mT", (128, 8, 512), BF16)
            xo = [sbuf(st, f"xo{i}", (128, 1024), F32) for i in range(2)]
            g = 0
            gx = 0
            for tb in range(NB):
                (ys_, Bys_), (ya_, Bya_), (gt_, Bgt_) = ysb[tb % 2], yab[tb % 2], gtb[tb % 2]
                tsl = slice(tb * 512, (tb + 1) * 512)
                sp.dma(ys_[:], s_ysT.rearrange("(k p) s -> p k s", p=128)[:, :, tsl], reads=[Bs_ysT], writes=[Bys_], own=Bys_)
                sp.dma(ya_[:], s_yaT.rearrange("(k p) s -> p k s", p=128)[:, :, tsl], reads=[Bs_yaT], writes=[Bya_], own=Bya_)
                sp.dma(gt_[:], s_gT.rearrange("(k p) s -> p k s", p=128)[:, :, tsl], reads=[Bs_gT], writes=[Bgt_], own=Bgt_)
                for cc in range(8):
                    ba, bb = (g % 2) * 2, (g % 2) * 2 + 1
                    g += 1
                    for kc in range(8):
                        pe.op(lambda: T_.matmul(P[:, ba, :], lhsT=Wbs[:, kc, cc * 128:(cc + 1) * 128], rhs=ys_[:, kc, :],
                                                start=(kc == 0), stop=(kc == 7)), [BWbs, Bys_], [BP[ba]])
                    for kc in range(8):
                        pe.op(lambda: T_.matmul(P[:, bb, :], lhsT=Wba[:, kc, cc * 128:(cc + 1) * 128], rhs=ya_[:, kc, :],
                                                start=(kc == 0), stop=(kc == 7)), [BWba, Bya_], [BP[bb]])
                    dve.op(lambda: V_.tensor_tensor(out=m1[:], in0=P[:, ba, :], in1=gt_[:, cc, :], op=ALU.mult), [BP[ba], Bgt_], [Bm1])
                    dve.op(lambda: V_.tensor_tensor(out=m2[:], in0=P[:, bb, :], in1=gt_[:, 8 + cc, :], op=ALU.mult), [BP[bb], Bgt_], [Bm2])
                    pool.op(lambda: G_.tensor_tensor(out=mT[:, cc, :], in0=m1[:], in1=m2[:], op=ALU.add), [Bm1, Bm2], [BmT])
                for tt in range(4):
                    t = tb * 4 + tt
                    xo_, Bxo_ = xo[gx % 2]
                    gx += 1
                    sp.dma(xo_[:], src[t * 128:(t + 1) * 128, :], reads=[Bsrc], writes=[Bxo_], own=Bxo_)
                    for dh in range(2):
                        bank = 4 + (2 * tt + dh) % 4
                        for cc in range(8):
                            pe.op(lambda: T_.matmul(P[:, bank, :], lhsT=mT[:, cc, tt * 128:(tt + 1) * 128],
                                                    rhs=Wo[:, cc, dh * 512:(dh + 1) * 512], start=(cc == 0), stop=(cc == 7)),
                                  [BmT, BWo], [BP[bank]])
                        dve.op(lambda: V_.tensor_tensor(out=xo_[:, dh * 512:(dh + 1) * 512], in0=P[:, bank, :],
                                                        in1=xo_[:, dh * 512:(dh + 1) * 512], op=ALU.add), [BP[bank], Bxo_], [Bxo_])
                    sp.dma(out[t * 128:(t + 1) * 128, :], xo_[:], reads=[Bxo_], writes=[Bout], own=Bxo_)
            fw.barrier()
            fw.release([Bsnw, Bsw1] + [b for _, b in wfsE + ysb + yab + gtb + xo])
        stE.close()
        if stop == "E":
            break

        is_moe = (L % 2 == 1)
        jx = L // 2
        comb, Bcomb = sbuf(top, f"comb{L}", (128, NT, 8), F32)
        with ExitStack() as st:
            hT, _ = sbuf(st, "hT", (128, 8, S), BF16)
            BhT = [Buf(f"hT{t}") for t in range(NT)]
            hook = None
            if is_moe:
                nfc, Bnfc = col_load(st, "nfc", I["norm_ffn_w"][L], 8)
                rwf, Brwf = sbuf(st, "rwf", (128, 8, 8), F32)
                sp.dma(rwf[:], I["router_w"][jx].rearrange("(k p) e -> p k e", p=128), reads=[Bin], writes=[Brwf], own=Brwf)
                dve.op(lambda: V_.tensor_tensor(out=rwf[:], in0=rwf[:], in1=nfc[:].unsqueeze(2).broadcast_to([128, 8, 8]), op=ALU.mult),
                       [Brwf, Bnfc], [Brwf])
                xnf2 = [sbuf(st, f"xnfF{i}", (128, 1024), F32) for i in range(2)]
                xTf2 = [sbuf(st, f"xTf{i}", (128, 8, 128), F32) for i in range(2)]
                lgA, BlgA = sbuf(st, "lgA", (128, NT, 8), F32)

                def hook(t, x_, Bx, ss_, Bss):
                    (xnf, Bxnf), (xTf, BxTf) = xnf2[t % 2], xTf2[t % 2]
                    pb = 4 + 2 * (t % 2)
                    dve.op(lambda: V_.tensor_scalar(out=xnf[:], in0=x_[:], scalar1=ss_[:, 0:1], scalar2=None, op0=ALU.mult),
                           [Bx, Bss], [Bxnf])
                    for kc in range(8):
                        pe.op(lambda: T_.transpose(out=P[:, pb + kc // 4, (kc % 4) * 128:(kc % 4 + 1) * 128],
                                                   in_=xnf[:, kc * 128:(kc + 1) * 128], identity=IDf), [Bxnf, Bcm], [BP[pb + kc // 4]])
                    act.op(lambda: A_.copy(out=xTf[:].rearrange("p (a b) t -> p a (b t)", a=2), in_=P[:, pb:pb + 2, :]),
                           [BP[pb], BP[pb + 1]], [BxTf])
                    for kc in range(8):
                        pe.op(lambda: T_.matmul(P[:, pb, 0:8], lhsT=xTf[:, kc, :], rhs=rwf[:, kc, :], start=(kc == 0), stop=(kc == 7)),
                              [BxTf, Brwf], [BP[pb]])
                    dve.op(lambda: V_.tensor_copy(out=lgA[:, t, :], in_=P[:, pb, 0:8]), [BP[pb]], [BlgA])
            rel = norm_T(st, out, Bout, hT, BhT, "nF", hook)
            if is_moe:
                l2A, Bl2A = sbuf(st, "l2A", (128, NT, 8), F32)
                mk1A, Bmk1A = sbuf(st, "mk1A", (128, NT, 8), F32)
                mk2A, Bmk2A = sbuf(st, "mk2A", (128, NT, 8), F32)
                mxA, BmxA = sbuf(st, "mxA", (128, 4, NT), F32)
                fl = lambda ap: ap.rearrange("p t e -> p (t e)")
                bc = lambda ap: ap.unsqueeze(2).broadcast_to([128, NT, 8])
                dve.op(lambda: V_.tensor_reduce(out=mxA[:, 0, :], in_=lgA[:], axis=AX.X, op=ALU.max), [BlgA], [BmxA])
                dve.op(lambda: V_.tensor_tensor(out=mk1A[:], in0=lgA[:], in1=bc(mxA[:, 0, :]), op=ALU.is_equal), [BlgA, BmxA], [Bmk1A])
                dve.op(lambda: V_.scalar_tensor_tensor(out=fl(l2A[:]), in0=fl(mk1A[:]), scalar=-1e30, in1=fl(lgA[:]), op0=ALU.mult,
                                                       op1=ALU.add), [Bmk1A, BlgA], [Bl2A])
                dve.op(lambda: V_.tensor_reduce(out=mxA[:, 1, :], in_=l2A[:], axis=AX.X, op=ALU.max), [Bl2A], [BmxA])
                dve.op(lambda: V_.tensor_tensor(out=mk2A[:], in0=l2A[:], in1=bc(mxA[:, 1, :]), op=ALU.is_equal), [Bl2A, BmxA], [Bmk2A])
                dve.op(lambda: V_.tensor_tensor(out=mxA[:, 2, :], in0=mxA[:, 0, :], in1=mxA[:, 1, :], op=ALU.subtract), [BmxA], [BmxA])
                act.op(lambda: A_.activation(out=mxA[:, 3, :], in_=mxA[:, 2, :], func=AF.Sigmoid, scale=-1.0), [BmxA], [BmxA])
                act.op(lambda: A_.activation(out=mxA[:, 2, :], in_=mxA[:, 2, :], func=AF.Sigmoid), [BmxA], [BmxA])
                dve.op(lambda: V_.tensor_tensor(out=mk1A[:], in0=mk1A[:], in1=bc(mxA[:, 2, :]), op=ALU.mult), [Bmk1A, BmxA], [Bmk1A])
                dve.op(lambda: V_.tensor_tensor(out=mk2A[:], in0=mk2A[:], in1=bc(mxA[:, 3, :]), op=ALU.mult), [Bmk2A, BmxA], [Bmk2A])
                dve.op(lambda: V_.tensor_tensor(out=comb[:], in0=mk2A[:], in1=mk1A[:], op=ALU.add), [Bmk2A, Bmk1A], [Bcomb])
            hTo, BhTo = sbuf(st, "hTo", (1, 2), F32)
            for kc in range(8):
                sp.dma(s_hT[kc * 128:(kc + 1) * 128, :], hT[:, kc, :], reads=BhT, writes=[Bs_hT], own=BhTo)
            fw.barrier()
            fw.release(rel + [BhTo] + ([Bnfc, Brwf] if is_moe else []))

        FB = min(1024, S)
        NFB = S // FB
        with ExitStack() as st:
            wfs = [sbuf(st, f"wfF{i}", (128, 8, 512), F32) for i in range(2)]
            wbs = [sbuf(st, f"wbF{i}", (128, 8, 512), BF16) for i in range(4)]
            nfc, Bnfc = col_load(st, "nfc2", I["norm_ffn_w"][L], 8)
            Wd, BWd = sbuf(st, "Wd", (128, NFF, 1024), BF16)
            HT, BHT = sbuf(st, "HT", (128, NFF, FB), BF16)
            hb, Bhb = sbuf(st, "hb", (128, 8, FB), BF16)
            sg, Bsg = sbuf(st, "sg", (128, 512), F32)
            xo = [sbuf(st, f"xoF{i}", (128, 1024), F32) for i in range(2)]
            gx = 0
            gw = 0
            gp = 0
            experts = list(range(NEXP)) if is_moe else [None]

            def w_aps(e):
                if is_moe:
                    return I["moe_w_gate"][jx][e], I["moe_w_up"][jx][e], I["moe_w_down"][jx][e]
                return I["ffn_w_gate"][jx], I["ffn_w_up"][jx], I["ffn_w_down"][jx]
            groups = [(e, fb, f0) for e in experts for fb in range(NFB) for f0 in range(0, NFF, 4)]
            gidx = {g_: i for i, g_ in enumerate(groups)}

            def issue_group(i):
                nonlocal_gw = rotw
                e_, fb_, f0_ = groups[i]
                nf_ = min(4, NFF - f0_)
                Wg_x, Wu_x, _ = w_aps(e_)
                res_ = []
                for W_a in (Wg_x, Wu_x):
                    wb_, Bwb_ = wbs[nonlocal_gw[0] % 4]
                    nonlocal_gw[0] += 1
                    cast_load(wfs, W_a[:, f0_ * 128:(f0_ + nf_) * 128].rearrange("(k p) c -> p k c", p=128), 8, nf_ * 128,
                              nfc[:, :], Bnfc, wb_[:, :, :nf_ * 128], Bwb_)
                    res_.append((wb_, Bwb_))
                return res_
            rotw = [0]
            pendg = {}
            for e in experts:
                Wg_a, Wu_a, Wd_a = w_aps(e)
                wd_jobs = [(f0, min(8, NFF - f0), hf) for f0 in range(0, NFF, 8) for hf in range(2)]
                for fb in range(NFB):
                    sp.dma(hb[:], s_hT.rearrange("(k p) s -> p k s", p=128)[:, :, fb * FB:(fb + 1) * FB], reads=[Bs_hT], writes=[Bhb], own=Bhb)
                    for f0 in range(0, NFF, 4):
                        nf = min(4, NFF - f0)
                        gi_ = gidx[(e, fb, f0)]
                        wgu = pendg.pop(gi_) if gi_ in pendg else issue_group(gi_)
                        if gi_ + 1 < len(groups):
                            pendg[gi_ + 1] = issue_group(gi_ + 1)
                        if fb == 0:
                            for _ in range(1 if f0 // 4 < 2 else 2):
                                if wd_jobs and f0 // 4 >= 1:
                                    wf0, wnk, whf = wd_jobs.pop(0)
                                    cast_load(wfs, Wd_a[wf0 * 128:(wf0 + wnk) * 128, whf * 512:(whf + 1) * 512].rearrange("(k p) c -> p k c", p=128),
                                              wnk, 512, None, None, Wd[:, wf0:wf0 + wnk, whf * 512:(whf + 1) * 512], BWd)
                        for fi in range(nf):
                            f = f0 + fi
                            for t5 in range(FB // 512):
                                bg, bu = (gp % 2) * 2, (gp % 2) * 2 + 1
                                gp += 1
                                for (wb_, Bwb_), bk in zip(wgu, (bg, bu)):
                                    for kc in range(8):
                                        pe.op(lambda: T_.matmul(P[:, bk, :], lhsT=wb_[:, kc, fi * 128:(fi + 1) * 128],
                                                                rhs=hb[:, kc, t5 * 512:(t5 + 1) * 512], start=(kc == 0), stop=(kc == 7)),
                                              [Bwb_, Bhb], [BP[bk]])
                                act.op(lambda: A_.activation(out=sg[:], in_=P[:, bg, :], func=AF.Silu), [BP[bg]], [Bsg])
                                dve.op(lambda: V_.tensor_tensor(out=HT[:, f, t5 * 512:(t5 + 1) * 512], in0=P[:, bu, :], in1=sg[:], op=ALU.mult),
                                       [BP[bu], Bsg], [BHT])
                    while fb == 0 and wd_jobs:
                        wf0, wnk, whf = wd_jobs.pop(0)
                        cast_load(wfs, Wd_a[wf0 * 128:(wf0 + wnk) * 128, whf * 512:(whf + 1) * 512].rearrange("(k p) c -> p k c", p=128),
                                  wnk, 512, None, None, Wd[:, wf0:wf0 + wnk, whf * 512:(whf + 1) * 512], BWd)
                    for tt in range(FB // 128):
                        t = fb * (FB // 128) + tt
                        xo_, Bxo_ = xo[gx % 2]
                        gx += 1
                        sp.dma(xo_[:], out[t * 128:(t + 1) * 128, :], reads=[Bout], writes=[Bxo_], own=Bxo_)
                        for dh in range(2):
                            bank = 4 + (2 * tt + dh) % 4
                            for f in range(NFF):
                                pe.op(lambda: T_.matmul(P[:, bank, :], lhsT=HT[:, f, tt * 128:(tt + 1) * 128],
                                                        rhs=Wd[:, f, dh * 512:(dh + 1) * 512], start=(f == 0), stop=(f == NFF - 1)),
                                      [BHT, BWd], [BP[bank]])
                            xs_ = xo_[:, dh * 512:(dh + 1) * 512]
                            if is_moe:
                                dve.op(lambda: V_.scalar_tensor_tensor(out=xs_, in0=P[:, bank, :], scalar=comb[:, t, e:e + 1], in1=xs_,
                                                                       op0=ALU.mult, op1=ALU.add), [BP[bank], Bcomb, Bxo_], [Bxo_])
                            else:
                                dve.op(lambda: V_.tensor_tensor(out=xs_, in0=P[:, bank, :], in1=xs_, op=ALU.add), [BP[bank], Bxo_], [Bxo_])
                        sp.dma(out[t * 128:(t + 1) * 128, :], xo_[:], reads=[Bxo_], writes=[Bout], own=Bxo_)
            fw.barrier()
            fw.release([Bnfc, Bhb] + [b for _, b in wfs + xo])

    fw.barrier()
    top.close()
    fw.stack.close()
    return nc


def kernel(**inputs):
    S = inputs["x"].shape[1]
    nc = build(S=S)
    consts = host_consts(S)
    in_maps = []
    for b in range(8):
        m = {"x": np.ascontiguousarray(inputs["x"][b])}
        for k in W_SHAPES:
            m[k] = np.ascontiguousarray(inputs[k])
        m.update(consts)
        in_maps.append(m)
    res = run_bass_kernel_spmd(nc, in_maps, core_ids=list(range(8)))
    return np.stack([r["out"] for r in res.results], 0)
```

```python
import math
import numpy as np
import ml_dtypes
import concourse.bass as bass
import concourse.mybir as mybir
from concourse.bass_utils import run_bass_kernel_spmd
from contextlib import ExitStack

F32 = mybir.dt.float32
BF16 = mybir.dt.bfloat16
AF = mybir.ActivationFunctionType
ALU = mybir.AluOpType
AX = mybir.AxisListType

D = 1024
IN_COLS = 7696
DFF = 2816
NFF = DFF // 128
NEXP = 8
EPS = 1e-6
O_Z, O_XBC, O_DT, O_Q, O_K, O_V, O_GS, O_GA = 0, 1024, 2560, 2576, 3600, 4624, 5648, 6672


class Sem:
    def __init__(self, h, name, is_dma):
        self.h, self.name, self.is_dma, self.total = h, name, is_dma, 0


class Buf:
    def __init__(self, name):
        self.name = name
        self.w = {}
        self.r = {}
        self.dsem = None
        self.dram = False


class Eng:
    def __init__(self, fw, name, eng, sem, self_sync):
        self.fw, self.name, self.e, self.sem, self.self_sync = fw, name, eng, sem, self_sync
        self.waited = {}

    def _wait(self, deps):
        for s, v in deps.items():
            if s is self.sem and not self.self_sync:
                continue
            if s.is_dma:
                v = s.total
            if self.waited.get(s, 0) >= v:
                continue
            self.e.wait_ge(s.h, v)
            self.waited[s] = v
            self.fw.n_waits += 1

    @staticmethod
    def _deps(reads, writes):
        deps = {}
        for b in reads:
            for s, v in b.w.items():
                if deps.get(s, 0) < v:
                    deps[s] = v
        for b in writes:
            for d in (b.w, b.r):
                for s, v in d.items():
                    if deps.get(s, 0) < v:
                        deps[s] = v
        return deps

    def op(self, fn, reads=(), writes=()):
        self._wait(self._deps(reads, writes))
        ins = fn()
        self.sem.total += 1
        ins.then_inc(self.sem.h, 1)
        tok = self.sem.total
        for b in reads:
            b.r[self.sem] = tok
        for b in writes:
            b.w = {self.sem: tok}
            b.r = {}
        self.fw.n_ins += 1
        return ins

    def dma(self, out_ap, in_ap, reads=(), writes=(), own=None, **kw):
        (dst,) = writes
        self._wait(self._deps(reads, writes))
        if own.dsem is None:
            own.dsem = self.fw.take_dma_sem()
        s = own.dsem
        ins = self.e.dma_start(out=out_ap, in_=in_ap, **kw)
        s.total += 16
        ins.then_inc(s.h, 16)
        for b in reads:
            b.r[s] = s.total
        if dst.dram:
            dst.w[s] = s.total
        else:
            if list(dst.w.keys()) == [s]:
                dst.w[s] = s.total
            else:
                dst.w = {s: s.total}
            dst.r = {}
        self.fw.n_dma += 1
        return ins


class FW:
    def __init__(self, nc, n_dma_sems=64):
        self.nc = nc
        self.n_waits = self.n_ins = self.n_dma = 0
        self.stack = ExitStack()
        self._free_dma = []
        self._sems = []
        for i in range(n_dma_sems):
            s = Sem(self.stack.enter_context(nc.semaphore(f"dq{i}")), f"dq{i}", True)
            self._free_dma.append(s)
            self._sems.append(s)

        def mk(name, eng, self_sync):
            s = Sem(self.stack.enter_context(nc.semaphore(f"e_{name}")), name, False)
            self._sems.append(s)
            return Eng(self, name, eng, s, self_sync)
        self.pe = mk("pe", nc.tensor, False)
        self.act = mk("act", nc.scalar, True)
        self.dve = mk("dve", nc.vector, True)
        self.pool = mk("pool", nc.gpsimd, True)
        self.sp = mk("sp", nc.sync, False)
        self.engs = (self.pe, self.act, self.dve, self.pool, self.sp)

    def take_dma_sem(self):
        return self._free_dma.pop()

    def release(self, bufs):
        for b in bufs:
            if b.dsem is not None:
                self._free_dma.insert(0, b.dsem)
                b.dsem = None

    def barrier(self):
        allv = {s: s.total for s in self._sems if s.total}
        for e in self.engs:
            e._wait(allv)


def host_consts(S):
    j = np.arange(128)
    same = (j[:, None] // 64) == (j[None, :] // 64)
    V = (same & (j[:, None] <= j[None, :])).astype(np.float32)
    U = (same & (j[:, None] > j[None, :])).astype(np.float32)
    OA = np.repeat((j < 64).astype(np.float32)[:, None], 128, 1)
    OB = np.repeat((j >= 64).astype(np.float32)[:, None], 128, 1)
    AD = (~((j[:, None] >= 64) & (j[None, :] < 64))).astype(np.float32)
    ID = np.eye(128, dtype=np.float32)
    ON = np.ones((128, 128), np.float32)
    masks = np.stack([V, U, OA, OB, AD, ID, ON], 1)
    inv = (500000.0 ** (-np.arange(0, 16, 2, dtype=np.float32) / 16)).astype(np.float32)
    ang = np.arange(S, dtype=np.float32)[:, None] * inv[None, :]
    rope = np.concatenate([np.cos(ang), np.sin(ang)], 1).astype(np.float32)
    return {"c_masks": np.ascontiguousarray(masks), "c_rope": rope}


W_SHAPES = {
    "norm_mix_w": (2, 1024), "w_in": (2, 1024, IN_COLS), "conv_w": (2, 4, 1536), "conv_b": (2, 1536),
    "dt_bias": (2, 16), "a_log": (2, 16), "d_skip": (2, 16), "ssd_norm_w": (2, 1024),
    "q_norm_w": (2, 64), "k_norm_w": (2, 64), "lambda_q1": (2, 64), "lambda_k1": (2, 64),
    "lambda_q2": (2, 64), "lambda_k2": (2, 64), "subln_w": (2, 128), "gate_b": (2, 2, 1024),
    "w_br_ssd": (2, 1024, 1024), "w_br_att": (2, 1024, 1024), "w_out": (2, 1024, 1024),
    "norm_ffn_w": (2, 1024), "ffn_w_gate": (1, 1024, DFF), "ffn_w_up": (1, 1024, DFF),
    "ffn_w_down": (1, DFF, 1024), "router_w": (1, 1024, 8), "moe_w_gate": (1, 8, 1024, DFF),
    "moe_w_up": (1, 8, 1024, DFF), "moe_w_down": (1, 8, DFF, 1024),
}


def build(S=4096, NL=2, debug=False, stop=None):
    NT = S // 128
    NB = S // 512
    nc = bass.Bass("TRN2", target_bir_lowering=False)
    fw = FW(nc)
    pe, act, dve, pool, sp = fw.pe, fw.act, fw.dve, fw.pool, fw.sp
    V_ = nc.vector
    A_ = nc.scalar
    G_ = nc.gpsimd
    T_ = nc.tensor

    def din(name, shape):
        return nc.dram_tensor(name, list(shape), F32, kind="ExternalInput").ap()
    I = {"x": din("x", (S, D))}
    for k, shp in W_SHAPES.items():
        I[k] = din(k, shp)
    c_masks = din("c_masks", (128, 7, 128))
    c_rope = din("c_rope", (S, 16))
    out = nc.dram_tensor("out", [S, D], F32, kind="ExternalOutput").ap()
    Bout = [Buf(f"out{t}") for t in range(NT)]
    for b_ in Bout:
        b_.dram = True

    def bsel(B, t):
        return B[t] if isinstance(B, list) else B
    Bin = Buf("in"); Bin.dram = True
    skind = "ExternalOutput" if debug else "Internal"

    def dscr(name, shape, dt):
        b = Buf(name); b.dram = True
        return nc.dram_tensor(name, list(shape), dt, kind=skind).ap(), b
    s_z, Bs_z = dscr("s_z", (S, D), BF16)
    s_q, Bs_q = dscr("s_q", (S, D), BF16)
    s_k, Bs_k = dscr("s_k", (S, D), BF16)
    s_v, Bs_v = dscr("s_v", (S, D), BF16)
    s_dt, Bs_dt = dscr("s_dt", (S, 16), F32)
    s_xbcT, Bs_xbcT = dscr("s_xbcT", (1536, S + 4), BF16)
    s_gT, Bs_gT = dscr("s_gT", (2048, S), BF16)
    s_qT, Bs_qT = dscr("s_qT", (8, 2, 128, S), BF16)
    s_kT, Bs_kT = dscr("s_kT", (8, 128, S), BF16)
    s_ysT, Bs_ysT = dscr("s_ysT", (D, S), BF16)
    s_yaT, Bs_yaT = dscr("s_yaT", (D, S), BF16)
    s_hT, Bs_hT = dscr("s_hT", (D, S), BF16)

    top = ExitStack()

    uniq = [0]

    def sbuf(st, name, shape, dt):
        uniq[0] += 1
        return st.enter_context(nc.sbuf_tensor(f"{name}_{uniq[0]}", list(shape), dt)), Buf(name)

    P = top.enter_context(nc.psum_tensor("P", [128, 8, 512], F32))
    BP = [Buf(f"P{i}") for i in range(8)]

    cm, Bcm = sbuf(top, "cm", (128, 7, 128), F32)
    cmb, Bcmb = sbuf(top, "cmb", (128, 7, 128), BF16)
    sp.dma(cm[:], c_masks[:, :, :], reads=[Bin], writes=[Bcm], own=Bcm)
    dve.op(lambda: V_.tensor_copy(out=cmb[:], in_=cm[:]), [Bcm], [Bcmb])
    Vf, Uf, OAf, OBf = cm[:, 0, :], cm[:, 1, :], cm[:, 2, :], cm[:, 3, :]
    ADb, IDb, ONb = cmb[:, 4, :], cmb[:, 5, :], cmb[:, 6, :]
    IDf = cm[:, 5, :]
    ONf = cm[:, 6, :]

    def rstd_from_ss(ss_ap, Bss, n):
        act.op(lambda: A_.activation(out=ss_ap, in_=ss_ap, func=AF.Sqrt, scale=1.0 / n, bias=EPS), [Bss], [Bss])
        dve.op(lambda: V_.reciprocal(out=ss_ap, in_=ss_ap), [Bss], [Bss])

    def norm_T(st, src, Bsrc, xT, BxT, tag, hook=None):
        NSL = 4
        xt = [sbuf(st, f"{tag}_x{i}", (128, D), F32) for i in range(NSL)]
        jks = [sbuf(st, f"{tag}_jk{i}", (128, D), BF16) for i in range(NSL)]
        ss = [sbuf(st, f"{tag}_ss{i}", (128, 1), F32) for i in range(NSL)]
        xn = [sbuf(st, f"{tag}_xn{i}", (128, D), BF16) for i in range(NSL)]

        def chain(t):
            i = t % NSL
            (x_, Bx), (ss_, Bss), (xn_, Bxn), (jk, Bjk) = xt[i], ss[i], xn[i], jks[i]
            sp.dma(x_[:], src[t * 128:(t + 1) * 128, :], reads=[bsel(Bsrc, t)], writes=[Bx], own=Bx)
            yield
            act.op(lambda: A_.activation(out=jk[:], in_=x_[:], func=AF.Square, accum_out=ss_[:]), [Bx], [Bjk, Bss])
            yield
            act.op(lambda: A_.activation(out=ss_[:], in_=ss_[:], func=AF.Sqrt, scale=1.0 / D, bias=EPS), [Bss], [Bss])
            yield
            dve.op(lambda: V_.reciprocal(out=ss_[:], in_=ss_[:]), [Bss], [Bss])
            yield
            dve.op(lambda: V_.tensor_scalar(out=xn_[:], in0=x_[:], scalar1=ss_[:, 0:1], scalar2=None, op0=ALU.mult),
                   [Bx, Bss], [Bxn])
            yield
            if hook is not None:
                hook(t, x_, Bx, ss_, Bss)
                yield
            bank = i
            pv = P[:, bank, :].bitcast(BF16)
            for kc in range(8):
                pe.op(lambda: T_.transpose(out=pv[:, kc * 128:(kc + 1) * 128], in_=xn_[:, kc * 128:(kc + 1) * 128],
                                           identity=IDb), [Bxn, Bcmb], [BP[bank]])
            yield
            (dve if t % 2 else act).op(
                (lambda: V_.tensor_copy(out=xT[:, :, t * 128:(t + 1) * 128], in_=pv.rearrange("p (k t) -> p k t", k=8)))
                if t % 2 else
                (lambda: A_.copy(out=xT[:, :, t * 128:(t + 1) * 128], in_=pv.rearrange("p (k t) -> p k t", k=8))),
                [BP[bank]], [BxT[t]])
            yield

        for t0 in range(0, NT, NSL):
            live = [chain(t) for t in range(t0, min(NT, t0 + NSL))]
            while live:
                for g_ in list(live):
                    try:
                        next(g_)
                    except StopIteration:
                        live.remove(g_)
        return [b for _, b in xt]

    def load_w(st_bufs, slot, src_rows_ap, ncols, scale_ap, Bscale):
        (wf, Bwf), (wb, Bwb) = st_bufs[slot]
        sp.dma(wf[:, :, :ncols], src_rows_ap.rearrange("(kc p) c -> p kc c", p=128), reads=[Bin], writes=[Bwf], own=Bwf)
        if scale_ap is None:
            pool.op(lambda: G_.tensor_copy(out=wb[:, :, :ncols], in_=wf[:, :, :ncols]), [Bwf], [Bwb])
        else:
            pool.op(lambda: G_.tensor_tensor(out=wb[:, :, :ncols], in0=wf[:, :, :ncols],
                                             in1=scale_ap.unsqueeze(2).broadcast_to([128, 8, ncols]), op=ALU.mult),
                    [Bwf, Bscale], [Bwb])
        return wb, Bwb

    def col_load(st, name, src_1d, n):
        t, B = sbuf(st, name, (128, n), F32)
        sp.dma(t[:], src_1d.rearrange("(c p) -> p c", p=128), reads=[Bin], writes=[B], own=B, allow_slow_non_contiguous=True)
        return t, B

    def bc_load(st, name, src_1d, n):
        t, B = sbuf(st, name, (128, n), F32)
        sp.dma(t[:], src_1d.partition_broadcast(128), reads=[Bin], writes=[B], own=B)
        return t, B

    evac_rr = [0]

    def evac_copy(out_ap, in_ap, reads, writes):
        evac_rr[0] ^= 1
        if evac_rr[0]:
            act.op(lambda: A_.copy(out=out_ap, in_=in_ap), reads, writes)
        else:
            dve.op(lambda: V_.tensor_copy(out=out_ap, in_=in_ap), reads, writes)

    wrot = [0, 0]

    def cast_load(wfs, src3, nk, ncols, scale_ap, Bscale, dst_ap, Bdst):
        wf, Bwf = wfs[wrot[0] % len(wfs)]
        wrot[0] += 1
        sp.dma(wf[:, :nk, :ncols], src3, reads=[Bin], writes=[Bwf], own=Bwf)
        if scale_ap is None:
            pool.op(lambda: G_.tensor_copy(out=dst_ap, in_=wf[:, :nk, :ncols]), [Bwf], [Bdst])
        else:
            pool.op(lambda: G_.tensor_tensor(out=dst_ap, in0=wf[:, :nk, :ncols],
                                             in1=scale_ap.unsqueeze(2).broadcast_to([128, nk, ncols]), op=ALU.mult),
                    [Bwf, Bscale], [Bdst])

    for L in range(NL):
        lam_init = 0.8 - 0.6 * math.exp(-0.3 * L)
        src, Bsrc = (I["x"], Bin) if L == 0 else (out, Bout)

        with ExitStack() as st:
            xT, _ = sbuf(st, "xT", (128, 8, S), BF16)
            BxT = [Buf(f"xT{t}") for t in range(NT)]
            rel = norm_T(st, src, Bsrc, xT, BxT, "nA")
            nw, Bnw = col_load(st, "nw", I["norm_mix_w"][L], 8)
            gb, Bgb = sbuf(st, "gb", (128, 16), F32)
            sp.dma(gb[:], I["gate_b"][L].rearrange("b (c p) -> p (b c)", p=128), reads=[Bin], writes=[Bgb], own=Bgb,
                   allow_slow_non_contiguous=True)
            wbufs = [(sbuf(st, f"wf{i}", (128, 8, 512), F32), sbuf(st, f"wb{i}", (128, 8, 512), BF16)) for i in range(2)]
            stg = [sbuf(st, f"stg{i}", (128, 512), BF16) for i in range(4)]
            stgf = [sbuf(st, f"stgf{i}", (128, 16), F32) for i in range(2)]
            zt, Bzt = sbuf(st, "zt", (128, 4), BF16)
            pool.op(lambda: G_.memset(zt[:], 0.0), [], [Bzt])
            for c in range(12):
                sp.dma(s_xbcT[c * 128:(c + 1) * 128, 0:4], zt[:], reads=[Bzt], writes=[Bs_xbcT], own=Bzt)
            blocks = []
            for (o, dst, Bd) in ((O_Q, s_q, Bs_q), (O_K, s_k, Bs_k), (O_Z, s_z, Bs_z), (O_V, s_v, Bs_v)):
                for h in range(2):
                    blocks.append((o + h * 512, 512, "tok", dst, Bd, h * 512))
            blocks.append((O_DT, 16, "dt", s_dt, Bs_dt, 0))
            for h in range(3):
                blocks.append((O_XBC + h * 512, 512, "xbc", s_xbcT, Bs_xbcT, h * 512))
            for h in range(4):
                blocks.append((O_GS + h * 512, 512, "gate", s_gT, Bs_gT, h * 512))
            stateB = {"bi": -1, "g": 0}

            def phaseB_gen():
                pend = load_w(wbufs, 0, I["w_in"][L][:, blocks[0][0]:blocks[0][0] + blocks[0][1]], blocks[0][1], nw[:, :], Bnw)
                for bi, (c0, ncol, kind, dst, Bd, d0) in enumerate(blocks):
                    stateB["bi"] = bi
                    wb, Bwb = pend
                    if bi + 1 < len(blocks):
                        c0n, ncn = blocks[bi + 1][0], blocks[bi + 1][1]
                        pend = load_w(wbufs, (bi + 1) % 2, I["w_in"][L][:, c0n:c0n + ncn], ncn, nw[:, :], Bnw)
                    if kind in ("tok", "dt"):
                        for t in range(NT):
                            g = stateB["g"]
                            bank = g % 4
                            for kc in range(8):
                                pe.op(lambda: T_.matmul(P[:, bank, :ncol], lhsT=xT[:, kc, t * 128:(t + 1) * 128], rhs=wb[:, kc, :ncol],
                                                        start=(kc == 0), stop=(kc == 7)), [BxT[t], Bwb], [BP[bank]])
                            if kind == "tok":
                                s_, Bs_ = stg[g % 4]
                                act.op(lambda: A_.copy(out=s_[:], in_=P[:, bank, :]), [BP[bank]], [Bs_])
                                sp.dma(dst[t * 128:(t + 1) * 128, d0:d0 + 512], s_[:], reads=[Bs_], writes=[Bd], own=Bs_)
                            else:
                                s_, Bs_ = stgf[g % 2]
                                act.op(lambda: A_.copy(out=s_[:], in_=P[:, bank, :16]), [BP[bank]], [Bs_])
                                sp.dma(dst[t * 128:(t + 1) * 128, :], s_[:], reads=[Bs_], writes=[Bd], own=Bs_)
                            stateB["g"] += 1
                            yield
                    else:
                        for cc in range(4):
                            for tb in range(NB):
                                g = stateB["g"]
                                bank = g % 4
                                for kc in range(8):
                                    pe.op(lambda: T_.matmul(P[:, bank, :], lhsT=wb[:, kc, cc * 128:(cc + 1) * 128],
                                                            rhs=xT[:, kc, tb * 512:(tb + 1) * 512], start=(kc == 0), stop=(kc == 7)),
                                          BxT[tb * 4:tb * 4 + 4] + [Bwb], [BP[bank]])
                                s_, Bs_ = stg[g % 4]
                                r0 = d0 + cc * 128
                                if kind == "xbc":
                                    act.op(lambda: A_.copy(out=s_[:], in_=P[:, bank, :]), [BP[bank]], [Bs_])
                                    sp.dma(dst[r0:r0 + 128, 4 + tb * 512:4 + (tb + 1) * 512], s_[:], reads=[Bs_], writes=[Bd], own=Bs_)
                                else:
                                    gi = r0 // 128
                                    act.op(lambda: A_.activation(out=s_[:], in_=P[:, bank, :], func=AF.Sigmoid, bias=gb[:, gi:gi + 1]),
                                           [BP[bank], Bgb], [Bs_])
                                    sp.dma(dst[r0:r0 + 128, tb * 512:(tb + 1) * 512], s_[:], reads=[Bs_], writes=[Bd], own=Bs_)
                                stateB["g"] += 1
                                yield

            rope, Brope = sbuf(st, "rope", (128, NT, 16), F32)
            sp.dma(rope[:], c_rope.rearrange("(t p) c -> p t c", p=128), reads=[Bin], writes=[Brope], own=Brope)
            wq, Bwq = bc_load(st, "wq", I["q_norm_w"][L], 64)
            wk, Bwk = bc_load(st, "wk", I["k_norm_w"][L], 64)
            dve.op(lambda: V_.tensor_scalar(out=wq[:], in0=wq[:], scalar1=0.125, scalar2=None, op0=ALU.mult), [Bwq], [Bwq])
            d1b = []
            for qi in range(2):
                d = {}
                d["xr"] = [sbuf(st, f"xr{qi}_{i}", (128, 16, 64), BF16) for i in range(2)]
                d["sq"] = sbuf(st, f"sq{qi}", (128, 16, 64), F32)
                d["s16"] = sbuf(st, f"s16{qi}", (128, 16), F32)
                d["xnf"] = sbuf(st, f"xnf{qi}", (128, 16, 64), F32)
                d["xb"] = sbuf(st, f"xb{qi}", (128, 16, 64), BF16)
                d["tmp"] = [sbuf(st, f"rt{qi}_{i}", (128, 16, 8), F32) for i in range(4)]
                d1b.append(d)
            xTs = [sbuf(st, f"xTs{i}", (128, 8, 128), BF16) for i in range(2)]
            xTq = [[sbuf(st, f"xTq{i}_{j}", (128, 8, 128), BF16) for j in range(2)] for i in range(2)]
            for i in range(2):
                for j in range(2):
                    pool.op(lambda: G_.memset(xTq[i][j][0][:], 0.0), [], [xTq[i][j][1]])

            def d1_gen(qi):
                srcd, Bsrcd, wt, Bwt, dstT, BdstT = ((s_q, Bs_q, wq, Bwq, s_qT, Bs_qT), (s_k, Bs_k, wk, Bwk, s_kT, Bs_kT))[qi]
                d = d1b[qi]
                (sq, Bsq), (s16, Bs16), (xnf, Bxnf), (xb_, Bxb_), tmp = d["sq"], d["s16"], d["xnf"], d["xb"], d["tmp"]
                for t in range(NT):
                    x_, Bx_ = d["xr"][t % 2]
                    sp.dma(x_[:].rearrange("p a b -> p (a b)"), srcd[t * 128:(t + 1) * 128, :], reads=[Bsrcd], writes=[Bx_], own=Bx_)
                    yield
                    dve.op(lambda: V_.tensor_tensor(out=sq[:], in0=x_[:], in1=x_[:], op=ALU.mult), [Bx_], [Bsq])
                    yield
                    dve.op(lambda: V_.tensor_reduce(out=s16[:], in_=sq[:], axis=AX.X, op=ALU.add), [Bsq], [Bs16])
                    yield
                    act.op(lambda: A_.activation(out=s16[:], in_=s16[:], func=AF.Sqrt, scale=1.0 / 64, bias=EPS), [Bs16], [Bs16])
                    yield
                    dve.op(lambda: V_.reciprocal(out=s16[:], in_=s16[:]), [Bs16], [Bs16])
                    yield
                    dve.op(lambda: V_.tensor_tensor(out=xnf[:], in0=x_[:], in1=s16[:].unsqueeze(2).broadcast_to([128, 16, 64]),
                                                    op=ALU.mult), [Bx_, Bs16], [Bxnf])
                    yield
                    pool.op(lambda: G_.tensor_tensor(out=xnf[:], in0=xnf[:], in1=wt[:].unsqueeze(1).broadcast_to([128, 16, 64]),
                                                     op=ALU.mult), [Bxnf, Bwt], [Bxnf])
                    yield
                    dve.op(lambda: V_.tensor_copy(out=xb_[:], in_=xnf[:]), [Bxnf], [Bxb_])
                    yield
                    cs = rope[:, t, 0:8].unsqueeze(1).broadcast_to([128, 16, 8])
                    sn = rope[:, t, 8:16].unsqueeze(1).broadcast_to([128, 16, 8])
                    r1, r2 = xnf[:, :, 0:8], xnf[:, :, 8:16]
                    for i, (a_, b_) in enumerate(((r1, cs), (r2, sn), (r2, cs), (r1, sn))):
                        dve.op(lambda: V_.tensor_tensor(out=tmp[i][0][:], in0=a_, in1=b_, op=ALU.mult), [Bxnf, Brope], [tmp[i][1]])
                        yield
                    dve.op(lambda: V_.tensor_tensor(out=xb_[:, :, 0:8], in0=tmp[0][0][:], in1=tmp[1][0][:], op=ALU.subtract),
                           [tmp[0][1], tmp[1][1]], [Bxb_])
                    yield
                    dve.op(lambda: V_.tensor_tensor(out=xb_[:, :, 8:16], in0=tmp[2][0][:], in1=tmp[3][0][:], op=ALU.add),
                           [tmp[2][1], tmp[3][1]], [Bxb_])
                    for _ in range(8):
                        yield
                    bank = 4 + 2 * qi + t % 2
                    pv = P[:, bank, :].bitcast(BF16)
                    for h in range(8):
                        pe.op(lambda: T_.transpose(out=pv[:, h * 128:(h + 1) * 128],
                                                   in_=xb_[:, 2 * h:2 * h + 2, :].rearrange("p a b -> p (a b)"), identity=IDb),
                              [Bxb_, Bcmb], [BP[bank]])
                    yield
                    pv3 = pv.rearrange("p (k t) -> p k t", k=8)
                    if qi == 0:
                        for j in range(2):
                            xq_, Bxq_ = xTq[t % 2][j]
                            evac_copy(xq_[j * 64:(j + 1) * 64], pv3[j * 64:(j + 1) * 64], [BP[bank]], [Bxq_])
                            sp.dma(s_qT[:, j].rearrange("h p s -> p h s")[:, :, t * 128:(t + 1) * 128], xq_[:], reads=[Bxq_],
                                   writes=[BdstT], own=Bxq_)
                    else:
                        xT_, BxT_ = xTs[t % 2]
                        evac_copy(xT_[:], pv3, [BP[bank]], [BxT_])
                        sp.dma(dstT.rearrange("h p s -> p h s")[:, :, t * 128:(t + 1) * 128], xT_[:], reads=[BxT_], writes=[BdstT], own=BxT_)
                    yield

            def zipper_w(pairs):
                live = [[g_, w_] for g_, w_ in pairs]
                while live:
                    for it in list(live):
                        for _ in range(it[1]):
                            try:
                                next(it[0])
                            except StopIteration:
                                live.remove(it)
                                break

            gB = phaseB_gen()
            while stateB["bi"] < 4:
                next(gB)
            zipper_w([(gB, 1), (d1_gen(0), 2), (d1_gen(1), 2)])
            fw.barrier()
            fw.release([Brope, Bwq, Bwk] + [b for _, b in d1b[0]["xr"] + d1b[1]["xr"] + xTs + xTq[0] + xTq[1]])
            fw.release(rel + [Bnw, Bgb, Bzt] + [b for (_, b) in stg] + [b for (_, b) in stgf] + [w[0][1] for w in wbufs])
        if stop == "B":
            break

        with ExitStack() as st:
            cw, Bcw = sbuf(st, "cw", (128, 4, 12), F32)
            for k in range(4):
                sp.dma(cw[:, k, :], I["conv_w"][L][k].rearrange("(c p) -> p c", p=128), reads=[Bin], writes=[Bcw], own=Bcw,
                       allow_slow_non_contiguous=True)
            cbc, Bcbc = col_load(st, "cbc", I["conv_b"][L], 12)
            cbrf, Bcbrf = sbuf(st, "cbrf", (1, 1280), F32)
            sp.dma(cbrf[:], I["conv_b"][L][0:1280].unsqueeze(0), reads=[Bin], writes=[Bcbrf], own=Bcbrf)
            cbr, Bcbr = sbuf(st, "cbr", (1, 1280), BF16)
            dve.op(lambda: V_.tensor_copy(out=cbr[:], in_=cbrf[:]), [Bcbrf], [Bcbr])
            diag, Bdiag = sbuf(st, "diag", (128, 12, 4, 128), BF16)
            for c in range(12):
                pool.op(lambda: G_.tensor_tensor(out=diag[:, c, :, :], in0=IDf.unsqueeze(1).broadcast_to([128, 4, 128]),
                                                 in1=cw[:, :, c].unsqueeze(2).broadcast_to([128, 4, 128]), op=ALU.mult),
                        [Bcm, Bcw], [Bdiag])
            dtb, Bdtb = bc_load(st, "dtb", I["dt_bias"][L], 16)
            aneg, Baneg = bc_load(st, "aneg", I["a_log"][L], 16)
            dsk, Bdsk = bc_load(st, "dsk", I["d_skip"][L], 16)
            act.op(lambda: A_.activation(out=aneg[:], in_=aneg[:], func=AF.Exp), [Baneg], [Baneg])
            dve.op(lambda: V_.tensor_scalar(out=aneg[:], in0=aneg[:], scalar1=-1.0, scalar2=None, op0=ALU.mult), [Baneg], [Baneg])
            hst, Bhst = sbuf(st, "hst", (128, 16, 64), F32)
            hbf = [sbuf(st, f"hbf{i}", (128, 1024), BF16) for i in range(2)]
            dve.op(lambda: V_.memset(hst[:], 0.0), [], [Bhst])
            dve.op(lambda: V_.memset(hbf[0][0][:], 0.0), [], [hbf[0][1]])
            Uw = [sbuf(st, f"Uw{i}", (128, 12, 132), BF16) for i in range(2)]
            dtr = [sbuf(st, f"dtr{i}", (128, 16), F32) for i in range(2)]
            ztl = [sbuf(st, f"ztl{i}", (128, 1024), BF16) for i in range(3)]
            xs2 = [sbuf(st, f"xs{i}", (128, 16, 64), BF16) for i in range(3)]
            Btk2 = [sbuf(st, f"Btk{i}", (128, 256), BF16) for i in range(2)]
            BCT2 = [sbuf(st, f"BCT{i}", (128, 4, 128), BF16) for i in range(2)]
            xdte2 = [sbuf(st, f"xdte{i}", (128, 16, 64), BF16) for i in range(2)]
            sm2 = [sbuf(st, f"sm{i}", (128, 64), F32) for i in range(2)]
            ydg2 = [sbuf(st, f"ydg{i}", (128, 16, 64), F32) for i in range(3)]
            t1s = [sbuf(st, f"t1s{i}", (128, 16, 64), F32) for i in range(2)]
            dt_, Bdt_ = sbuf(st, "dt_", (128, 16), F32)
            adt, Badt = sbuf(st, "adt", (128, 16), F32)
            R_, BR_ = sbuf(st, "R_", (128, 16, 128), F32)
            Es, BEs = sbuf(st, "Es", (128, 16, 128), F32)
            cbm, Bcbm = sbuf(st, "cbm", (128, 2, 128), F32)
            MT, BMT = sbuf(st, "MT", (128, 16, 128), BF16)
            dtd, Bdtd = sbuf(st, "dtd", (128, 16), F32)
            xdt, Bxdt = sbuf(st, "xdt", (128, 16, 64), BF16)
            t1, Bt1 = sbuf(st, "t1", (128, 16, 64), F32)
            t2, Bt2 = sbuf(st, "t2", (128, 16, 64), F32)
            sz, Bsz = sbuf(st, "sz", (128, 1024), F32)
            jk, Bjk = sbuf(st, "jkC", (128, 512), F32)
            ss2, Bss2 = sbuf(st, "ss2", (128, 2), F32)
            yn, Byn = sbuf(st, "yn", (128, 1024), BF16)
            yTs = [sbuf(st, f"yTs{i}", (128, 8, 128), BF16) for i in range(2)]
            xbcT_v = s_xbcT.rearrange("(c p) s -> p c s", p=128)
            ysT_v = s_ysT.rearrange("(k p) s -> p k s", p=128)

            NS = NT * 16
            dtA, BdtA = sbuf(st, "dtA", (128, NT, 16), F32)
            adtA, BadtA = sbuf(st, "adtA", (128, NT, 16), F32)
            smA, BsmA = sbuf(st, "smA", (128, 4, NS), F32)
            dtdA, BdtdA = sbuf(st, "dtdA", (128, NT, 16), F32)
            sp.dma(dtA[:], s_dt.rearrange("(t p) h -> p t h", p=128), reads=[Bs_dt], writes=[BdtA], own=BdtA)
            dve.op(lambda: V_.tensor_tensor(out=dtA[:], in0=dtA[:], in1=dtb[:].unsqueeze(1).broadcast_to([128, NT, 16]), op=ALU.add),
                   [BdtA, Bdtb], [BdtA])
            act.op(lambda: A_.activation(out=dtA[:], in_=dtA[:], func=AF.Exp), [BdtA], [BdtA])
            act.op(lambda: A_.activation(out=dtA[:], in_=dtA[:], func=AF.Ln, bias=1.0), [BdtA], [BdtA])
            dve.op(lambda: V_.tensor_tensor(out=adtA[:], in0=dtA[:], in1=aneg[:].unsqueeze(1).broadcast_to([128, NT, 16]), op=ALU.mult),
                   [BdtA, Baneg], [BadtA])
            for i, m_ in enumerate((Vf, Uf, OAf, OBf)):
                pe.op(lambda: T_.matmul(P[:, i, 0:NS], lhsT=m_, rhs=adtA[:].rearrange("p t h -> p (t h)"), start=True, stop=True),
                      [Bcm, BadtA], [BP[i]])
            act.op(lambda: A_.activation(out=smA[:], in_=P[:, 0:4, 0:NS], func=AF.Exp), [BP[0], BP[1], BP[2], BP[3]], [BsmA])
            dve.op(lambda: V_.tensor_tensor(out=dtdA[:].rearrange("p t h -> p (t h)"), in0=dtA[:].rearrange("p t h -> p (t h)"),
                                            in1=smA[:, 1, :], op=ALU.mult), [BdtA, BsmA], [BdtdA])

            def ssd_stage1(t):
                i2 = t % 2
                i3 = t % 3
                b0 = 4 * i2
                (U_, BU_), (dtr_, Bdtr_), (zt_, Bzt_) = Uw[i2], dtr[i2], ztl[i3]
                (xs, Bxs), (Btk, BBtk), (BCT, BBCT) = xs2[i3], Btk2[i2], BCT2[i2]
                (xdte, Bxdte), (sm, Bsm), (ydg, Bydg) = xdte2[i2], sm2[i2], ydg2[i3]
                sp.dma(U_[:, :, 0:131], xbcT_v[:, :, t * 128 + 1:t * 128 + 132], reads=[Bs_xbcT], writes=[BU_], own=BU_)
                sp.dma(zt_[:], s_z[t * 128:(t + 1) * 128, :], reads=[Bs_z], writes=[Bzt_], own=Bzt_)
                yield
                for c in range(10):
                    bank, col = (b0 + c // 4, (c % 4) * 128) if c < 8 else (b0 + 3, (c - 8) * 128)
                    o_ = P[:, bank, col:col + 128]
                    for k in range(4):
                        pe.op(lambda: T_.matmul(o_, lhsT=U_[:, c, k:k + 128], rhs=diag[:, c, k, :], start=(k == 0), stop=False),
                              [BU_, Bdiag], [BP[bank]])
                    pe.op(lambda: T_.matmul(o_, lhsT=ONb[0:1, :], rhs=cbr[0:1, c * 128:(c + 1) * 128], start=False, stop=True),
                          [Bcmb, Bcbr], [BP[bank]])
                    yield
                for i, c in enumerate(range(8, 12)):
                    for k in range(4):
                        pe.op(lambda: T_.matmul(P[:, b0 + 2, i * 128:(i + 1) * 128], lhsT=diag[:, c, k, :], rhs=U_[:, c, k:k + 128],
                                                start=(k == 0), stop=(k == 3)), [BU_, Bdiag], [BP[b0 + 2]])
                    yield
                act.op(lambda: A_.activation(out=xs[:].rearrange("p (b h) d -> p b (h d)", b=2), in_=P[:, b0:b0 + 2, :], func=AF.Silu),
                       [BP[b0], BP[b0 + 1]], [Bxs])
                act.op(lambda: A_.activation(out=Btk[:], in_=P[:, b0 + 3, 0:256], func=AF.Silu), [BP[b0 + 3]], [BBtk])
                yield
                for i in range(4):
                    act.op(lambda: A_.activation(out=BCT[:, i, :], in_=P[:, b0 + 2, i * 128:(i + 1) * 128], func=AF.Silu,
                                                 bias=cbc[:, 8 + i:9 + i]), [BP[b0 + 2], Bcbc], [BBCT])
                yield
                for g in range(2):
                    pe.op(lambda: T_.matmul(P[:, b0 + 3, 256 + g * 128:256 + (g + 1) * 128], lhsT=BCT[:, g, :], rhs=BCT[:, 2 + g, :],
                                            start=True, stop=True), [BBCT], [BP[b0 + 3]])
                dve.op(lambda: V_.tensor_tensor(out=cbm[:], in0=P[:, b0 + 3, 256:512].rearrange("p (g l) -> p g l", g=2),
                                                in1=Vf.unsqueeze(1).broadcast_to([128, 2, 128]), op=ALU.mult),
                       [BP[b0 + 3], Bcm], [Bcbm])
                yield
                dve.op(lambda: V_.tensor_tensor(out=xdt[:], in0=xs[:], in1=dtA[:, t, :].unsqueeze(2).broadcast_to([128, 16, 64]),
                                                op=ALU.mult), [Bxs, BdtA], [Bxdt])
                yield
                pool.op(lambda: G_.tensor_tensor(out=xdte[:], in0=xs[:], in1=dtdA[:, t, :].unsqueeze(2).broadcast_to([128, 16, 64]),
                                                 op=ALU.mult), [Bxs, BdtdA], [Bxdte])
                dve.op(lambda: V_.tensor_tensor(out=R_[:], in0=adtA[:, t, :].unsqueeze(2).broadcast_to([128, 16, 128]),
                                                in1=Vf.unsqueeze(1).broadcast_to([128, 16, 128]), op=ALU.mult),
                       [BadtA, Bcm], [BR_])
                yield
                for hq in range(4):
                    pe.op(lambda: T_.matmul(P[:, b0 + hq, :], lhsT=Uf, rhs=R_[:, hq * 4:(hq + 1) * 4, :].rearrange("p a b -> p (a b)"),
                                            start=True, stop=True), [Bcm, BR_], [BP[b0 + hq]])
                    yield
                act.op(lambda: A_.activation(out=Es[:].rearrange("p (a b) l -> p a (b l)", a=4), in_=P[:, b0:b0 + 4, :], func=AF.Exp),
                       [BP[b0], BP[b0 + 1], BP[b0 + 2], BP[b0 + 3]], [BEs])
                yield
                dve.op(lambda: V_.tensor_tensor(out=MT[:].rearrange("p (g e) l -> p g e l", g=2),
                                                in0=Es[:].rearrange("p (g e) l -> p g e l", g=2),
                                                in1=cbm[:].unsqueeze(2).broadcast_to([128, 2, 8, 128]), op=ALU.mult),
                       [BEs, Bcbm], [BMT])
                yield
                for h in range(16):
                    pe.op(lambda: T_.matmul(P[:, b0 + h // 8, (h % 8) * 64:(h % 8 + 1) * 64], lhsT=MT[:, h, :], rhs=xdt[:, h, :],
                                            start=True, stop=True), [BMT, Bxdt], [BP[b0 + h // 8]])
                    if h % 4 == 3:
                        yield
                evac_copy(ydg[:].rearrange("p (b h) d -> p b (h d)", b=2), P[:, b0:b0 + 2, :], [BP[b0], BP[b0 + 1]], [Bydg])
                yield

            def ssd_stage2(t):
                i2 = t % 2
                b0 = 4 * i2
                (Btk, BBtk), (BCT, BBCT), (xdte, Bxdte) = Btk2[i2], BCT2[i2], xdte2[i2]
                t1, Bt1 = t1s[i2]
                for ch in range(2):
                    r0 = ch * 64
                    (hb_in, Bhb_in), (hb_out, Bhb_out) = hbf[ch], hbf[1 - ch]
                    for g in range(2):
                        pe.op(lambda: T_.matmul(P[r0:r0 + 64, b0 + g, :], lhsT=BCT[:, 2 + g, r0:r0 + 64],
                                                rhs=hb_in[:, g * 512:(g + 1) * 512], start=True, stop=True),
                              [BBCT, Bhb_in], [BP[b0 + g]])
                    for g in range(2):
                        pe.op(lambda: T_.matmul(P[:, b0 + 2 + g, :], lhsT=Btk[r0:r0 + 64, g * 128:(g + 1) * 128],
                                                rhs=xdte[r0:r0 + 64, g * 8:(g + 1) * 8, :].rearrange("p a b -> p (a b)"),
                                                start=True, stop=True), [BBtk, Bxdte], [BP[b0 + 2 + g]])
                    yield
                    cd = smA[:, 2 + ch, t * 16:(t + 1) * 16]
                    dve.op(lambda: V_.tensor_tensor(out=hst[:], in0=hst[:], in1=cd.unsqueeze(2).broadcast_to([128, 16, 64]),
                                                    op=ALU.mult), [Bhst, BsmA], [Bhst])
                    yield
                    dve.op(lambda: V_.tensor_tensor(out=hst[:].rearrange("p (b h) d -> p b (h d)", b=2),
                                                    in0=hst[:].rearrange("p (b h) d -> p b (h d)", b=2), in1=P[:, b0 + 2:b0 + 4, :],
                                                    op=ALU.add), [Bhst, BP[b0 + 2], BP[b0 + 3]], [Bhst])
                    yield
                    act.op(lambda: A_.copy(out=hb_out[:], in_=hst[:].rearrange("p h d -> p (h d)")), [Bhst], [Bhb_out])
                    yield
                dve.op(lambda: V_.tensor_tensor(out=t1[:], in0=P[:, b0:b0 + 2, :].rearrange("p b (h d) -> p (b h) d", d=64),
                                                in1=smA[:, 0, t * 16:(t + 1) * 16].unsqueeze(2).broadcast_to([128, 16, 64]), op=ALU.mult),
                       [BP[b0], BP[b0 + 1], BsmA], [Bt1])
                yield

            def ssd_stage3(t):
                i2 = t % 2
                i3 = t % 3
                b0 = 4 * i2
                (zt_, Bzt_), (xs, Bxs), (ydg, Bydg) = ztl[i3], xs2[i3], ydg2[i3]
                t1, Bt1 = t1s[i2]
                pool.op(lambda: G_.tensor_tensor(out=t2[:], in0=xs[:], in1=dsk[:].unsqueeze(2).broadcast_to([128, 16, 64]),
                                                 op=ALU.mult), [Bxs, Bdsk], [Bt2])
                yield
                dve.op(lambda: V_.tensor_tensor(out=t1[:], in0=t1[:], in1=ydg[:], op=ALU.add), [Bt1, Bydg], [Bt1])
                act.op(lambda: A_.activation(out=sz[:], in_=zt_[:], func=AF.Silu), [Bzt_], [Bsz])
                yield
                dve.op(lambda: V_.tensor_tensor(out=t1[:], in0=t1[:], in1=t2[:], op=ALU.add), [Bt1, Bt2], [Bt1])
                yield
                dve.op(lambda: V_.tensor_tensor(out=t1[:].rearrange("p h d -> p (h d)"), in0=t1[:].rearrange("p h d -> p (h d)"),
                                                in1=sz[:], op=ALU.mult), [Bt1, Bsz], [Bt1])
                yield
                t1f = t1[:].rearrange("p h d -> p (h d)")
                for g in range(2):
                    act.op(lambda: A_.activation(out=jk[:], in_=t1f[:, g * 512:(g + 1) * 512], func=AF.Square,
                                                 accum_out=ss2[:, g:g + 1]), [Bt1], [Bjk, Bss2])
                    yield
                act.op(lambda: A_.activation(out=ss2[:], in_=ss2[:], func=AF.Sqrt, scale=1.0 / 512, bias=EPS), [Bss2], [Bss2])
                yield
                dve.op(lambda: V_.reciprocal(out=ss2[:], in_=ss2[:]), [Bss2], [Bss2])
                yield
                for g in range(2):
                    pool.op(lambda: G_.tensor_scalar(out=yn[:, g * 512:(g + 1) * 512], in0=t1f[:, g * 512:(g + 1) * 512],
                                                     scalar1=ss2[:, g:g + 1], scalar2=None, op0=ALU.mult), [Bt1, Bss2], [Byn])
                yield

            def ssd_stage4(t):
                bank = 4 * ((t + 1) % 2) + 2
                pv = P[:, bank, :].bitcast(BF16)
                for kc in range(8):
                    pe.op(lambda: T_.transpose(out=pv[:, kc * 128:(kc + 1) * 128], in_=yn[:, kc * 128:(kc + 1) * 128],
                                               identity=IDb), [Byn, Bcmb], [BP[bank]])
                yT_, ByT_ = yTs[t % 2]
                evac_copy(yT_[:], pv.rearrange("p (k t) -> p k t", k=8), [BP[bank]], [ByT_])
                sp.dma(ysT_v[:, :, t * 128:(t + 1) * 128], yT_[:], reads=[ByT_], writes=[Bs_ysT], own=ByT_)

            def zipper(*gens):
                live = [g_ for g_ in gens if g_ is not None]
                while live:
                    for g_ in list(live):
                        try:
                            next(g_)
                        except StopIteration:
                            live.remove(g_)

            zipper(ssd_stage1(0))
            for t in range(NT + 1):
                zipper(ssd_stage1(t + 1) if t + 1 < NT else None,
                       ssd_stage2(t) if t < NT else None,
                       ssd_stage3(t - 1) if t >= 1 else None)
                if t >= 1:
                    ssd_stage4(t - 1)
            fw.barrier()
            fw.release([Bcw, Bcbc, Bcbrf, Bdtb, Baneg, Bdsk, BdtA] + [b for _, b in Uw + dtr + ztl + yTs])
        if stop == "C":
            break

        stE = ExitStack()
        wfsE = [sbuf(stE, f"wfE{i}", (128, 8, 512), F32) for i in range(2)]
        snw, Bsnw = col_load(stE, "snw", I["ssd_norm_w"][L], 8)
        sw1, Bsw1 = col_load(stE, "sw1", I["subln_w"][L], 1)
        sw8, Bsw8 = sbuf(stE, "sw8", (128, 8), F32)
        dve.op(lambda: V_.tensor_scalar(out=sw8[:], in0=sw1[:, 0:1].broadcast_to([128, 8]), scalar1=1.0 - lam_init, scalar2=None,
                                        op0=ALU.mult), [Bsw1], [Bsw8])
        Wr = [sbuf(stE, f"Wr{i}", (128, 8, 1024), BF16) for i in range(3)]

        def prefetch_E():
            for wi, (nm, sc, Bsc) in enumerate((("w_br_ssd", snw, Bsnw), ("w_br_att", sw8, Bsw8), ("w_out", None, None))):
                for hf in range(2):
                    cast_load(wfsE, I[nm][L][:, hf * 512:(hf + 1) * 512].rearrange("(k p) c -> p k c", p=128), 8, 512,
                              None if sc is None else sc[:, :], Bsc, Wr[wi][0][:, :, hf * 512:(hf + 1) * 512], Wr[wi][1])

        with ExitStack() as st:
            lv = [bc_load(st, f"lv{i}", I[n][L], 64) for i, n in enumerate(("lambda_q1", "lambda_k1", "lambda_q2", "lambda_k2"))]
            lp, Blp = sbuf(st, "lp", (128, 64), F32)
            le, Ble = sbuf(st, "le", (128, 2), F32)
            nlam, Bnlam = sbuf(st, "nlam", (128, 1), F32)
            for i in range(2):
                dve.op(lambda: V_.tensor_tensor(out=lp[:], in0=lv[2 * i][0][:], in1=lv[2 * i + 1][0][:], op=ALU.mult),
                       [lv[2 * i][1], lv[2 * i + 1][1]], [Blp])
                dve.op(lambda: V_.tensor_reduce(out=le[:, i:i + 1], in_=lp[:], axis=AX.X, op=ALU.add), [Blp], [Ble])
            act.op(lambda: A_.activation(out=le[:], in_=le[:], func=AF.Exp), [Ble], [Ble])
            dve.op(lambda: V_.tensor_tensor(out=nlam[:], in0=le[:, 1:2], in1=le[:, 0:1], op=ALU.subtract), [Ble], [Bnlam])
            dve.op(lambda: V_.tensor_scalar(out=nlam[:], in0=nlam[:], scalar1=-lam_init, scalar2=None, op0=ALU.add), [Bnlam], [Bnlam])
            QT = [sbuf(st, f"QT{i}", (128, 2, S), BF16) for i in range(2)]
            KT = [sbuf(st, f"KT{i}", (128, S), BF16) for i in range(2)]
            Vh = [sbuf(st, f"Vh{i}", (128, NT, 130), BF16) for i in range(2)]
            PT = [sbuf(st, f"PT{i}", (128, 512), BF16) for i in range(4)]
            accs = [sbuf(st, f"accs{i}", (128, 4, 386), F32) for i in range(2)]
            rs, Brs = sbuf(st, "rs", (128, 4, 2), F32)
            c1, Bc1 = sbuf(st, "c1", (128, 4), F32)
            t0, Bt0 = sbuf(st, "t0", (128, 4, 128), F32)
            t1, Bt1 = sbuf(st, "t1D", (128, 4, 128), F32)
            ssd_, Bssd = sbuf(st, "ssd", (128, 4), F32)
            yb, Byb = sbuf(st, "yb", (128, 4, 128), BF16)
            yaTs = [sbuf(st, f"yaTs{i}", (128, 512), BF16) for i in range(2)]
            for i in range(2):
                pool.op(lambda: G_.memset(Vh[i][0][:, :, 128:130], 1.0), [], [Vh[i][1]])
            LOOK = 3
            rot = {"gs": 0, "ge": 0, "pt": 0}

            def emit_scores(stp):
                h, qb, kt, j, QT_, BQT_, KT_, BKT_, Vh_, BVh_ = stp["a"]
                i = kt - 4 * qb
                qlo = 0 if i < 0 else i * 128
                sb = rot["gs"] % 4
                rot["gs"] += 1
                PT_, BPT_ = PT[rot["pt"] % 4]
                rot["pt"] += 1
                stp["pt"] = (PT_, BPT_)
                pe.op(lambda: T_.matmul(P[:, sb, qlo:512], lhsT=KT_[:, kt * 128:(kt + 1) * 128],
                                        rhs=QT_[:, j, qb * 512 + qlo:(qb + 1) * 512], start=True, stop=True),
                      [BKT_, BQT_], [BP[sb]])
                act.op(lambda: A_.activation(out=PT_[:, qlo:512], in_=P[:, sb, qlo:512], func=AF.Exp), [BP[sb]], [BPT_])
                if i >= 0:
                    pool.op(lambda: G_.tensor_tensor(out=PT_[:, qlo:qlo + 128], in0=PT_[:, qlo:qlo + 128], in1=ADb, op=ALU.mult),
                            [BPT_, Bcmb], [BPT_])

            def emit_av(stp):
                h, qb, kt, j, QT_, BQT_, KT_, BKT_, Vh_, BVh_ = stp["a"]
                i = kt - 4 * qb
                PT_, BPT_ = stp["pt"]
                for s_ in range(max(i, 0), 4):
                    first = (kt == 0 and j == 0)
                    last = (kt == 4 * qb + s_) and j == 1
                    pe.op(lambda: T_.matmul(P[:, 4 + s_, j * 256:j * 256 + 129], lhsT=PT_[:, s_ * 128:(s_ + 1) * 128],
                                            rhs=Vh_[:, kt, 0:129], start=first, stop=last, skip_group_check=True),
                          [BPT_, BVh_], [BP[4 + s_]])
                if kt == 4 * qb + 3 and j == 1:
                    emit_epi(h, qb)

            def emit_epi(h, qb):
                ac, Bac = accs[rot["ge"] % 2]
                ya_, Bya_ = yaTs[rot["ge"] % 2]
                rot["ge"] += 1
                dve.op(lambda: V_.tensor_copy(out=ac[:, :, 0:385], in_=P[:, 4:8, 0:385]), [BP[4], BP[5], BP[6], BP[7]], [Bac])
                dve.op(lambda: V_.reciprocal(out=rs[:], in_=ac[:, :, 128:385:256]), [Bac], [Brs])
                dve.op(lambda: V_.tensor_tensor(out=c1[:], in0=rs[:, :, 1], in1=nlam[:, 0:1].broadcast_to([128, 4]), op=ALU.mult),
                       [Brs, Bnlam], [Bc1])
                dve.op(lambda: V_.tensor_tensor(out=t0[:], in0=ac[:, :, 0:128], in1=rs[:, :, 0:1].broadcast_to([128, 4, 128]), op=ALU.mult),
                       [Bac, Brs], [Bt0])
                dve.op(lambda: V_.tensor_tensor(out=t1[:], in0=ac[:, :, 256:384], in1=c1[:].unsqueeze(2).broadcast_to([128, 4, 128]),
                                                op=ALU.mult), [Bac, Bc1], [Bt1])
                dve.op(lambda: V_.tensor_tensor(out=t0[:], in0=t0[:], in1=t1[:], op=ALU.add), [Bt0, Bt1], [Bt0])
                dve.op(lambda: V_.tensor_tensor(out=t1[:], in0=t0[:], in1=t0[:], op=ALU.mult), [Bt0], [Bt1])
                dve.op(lambda: V_.tensor_reduce(out=ssd_[:], in_=t1[:], axis=AX.X, op=ALU.add), [Bt1], [Bssd])
                act.op(lambda: A_.activation(out=ssd_[:], in_=ssd_[:], func=AF.Ln, scale=1.0 / 128, bias=EPS), [Bssd], [Bssd])
                act.op(lambda: A_.activation(out=ssd_[:], in_=ssd_[:], func=AF.Exp, scale=-0.5), [Bssd], [Bssd])
                dve.op(lambda: V_.tensor_tensor(out=yb[:], in0=t0[:], in1=ssd_[:].unsqueeze(2).broadcast_to([128, 4, 128]), op=ALU.mult),
                       [Bt0, Bssd], [Byb])
                eb = rot["gs"] % 4
                rot["gs"] += 1
                pv = P[:, eb, :].bitcast(BF16)
                for s_ in range(4):
                    pe.op(lambda: T_.transpose(out=pv[:, s_ * 128:(s_ + 1) * 128], in_=yb[:, s_, :], identity=IDb), [Byb, Bcmb], [BP[eb]])
                dve.op(lambda: V_.tensor_copy(out=ya_[:], in_=pv[:, 0:512]), [BP[eb]], [Bya_])
                sp.dma(s_yaT[h * 128:(h + 1) * 128, qb * 512:(qb + 1) * 512], ya_[:], reads=[Bya_], writes=[Bs_yaT], own=Bya_)

            def load_head(h):
                (QT_, BQT_), (KT_, BKT_), (Vh_, BVh_) = QT[h % 2], KT[h % 2], Vh[h % 2]
                sp.dma(QT_[:], s_qT[h].rearrange("j p s -> p j s"), reads=[Bs_qT], writes=[BQT_], own=BQT_)
                sp.dma(KT_[:], s_kT[h], reads=[Bs_kT], writes=[BKT_], own=BKT_)
                sp.dma(Vh_[:, :, 0:128], s_v.rearrange("(t p) c -> p t c", p=128)[:, :, h * 128:(h + 1) * 128],
                       reads=[Bs_v], writes=[BVh_], own=BVh_)

            steps = []
            for h in range(8):
                (QT_, BQT_), (KT_, BKT_), (Vh_, BVh_) = QT[h % 2], KT[h % 2], Vh[h % 2]
                for qb in range(NB):
                    for kt in range(4 * qb + 4):
                        for j in range(2):
                            steps.append({"a": (h, qb, kt, j, QT_, BQT_, KT_, BKT_, Vh_, BVh_), "load": (qb == 0 and kt == 0 and j == 0)})
            for n in range(len(steps) + LOOK):
                if n < len(steps):
                    stp = steps[n]
                    if stp["load"] and stp["a"][0] == 0:
                        for h in (0, 1):
                            load_head(h)
                        prefetch_E()
                    emit_scores(stp)
                if n - LOOK >= 0:
                    sm_ = steps[n - LOOK]
                    if sm_["load"] and 1 <= sm_["a"][0] <= 6:
                        load_head(sm_["a"][0] + 1)
                    emit_av(sm_)
            fw.barrier()
            fw.release([b for _, b in lv + QT + KT + Vh + yaTs])
        if stop == "D":
            stE.close()
            break

        with ExitStack() as st:
            (Wbs, BWbs), (Wba, BWba), (Wo, BWo) = Wr
            ysb = [sbuf(st, f"ysb{i}", (128, 8, 512), BF16) for i in range(2)]
            yab = [sbuf(st, f"yab{i}", (128, 8, 512), BF16) for i in range(2)]
            gtb = [sbuf(st, f"gtb{i}", (128, 16, 512), BF16) for i in range(2)]
            m1, Bm1 = sbuf(st, "m1", (128, 512), F32)
            m2, Bm2 = sbuf(st, "m2", (128, 512), F32)
            mT, BmT = sbuf(st, "mT", (128, 8, 512), BF16)
            xo = [sbuf(st, f"xo{i}", (128, 1024), F32) for i in range(2)]
            g = 0
            gx = 0
            for tb in range(NB):
                (ys_, Bys_), (ya_, Bya_), (gt_, Bgt_) = ysb[tb % 2], yab[tb % 2], gtb[tb % 2]
                tsl = slice(tb * 512, (tb + 1) * 512)
                sp.dma(ys_[:], s_ysT.rearrange("(k p) s -> p k s", p=128)[:, :, tsl], reads=[Bs_ysT], writes=[Bys_], own=Bys_)
                sp.dma(ya_[:], s_yaT.rearrange("(k p) s -> p k s", p=128)[:, :, tsl], reads=[Bs_yaT], writes=[Bya_], own=Bya_)
                sp.dma(gt_[:], s_gT.rearrange("(k p) s -> p k s", p=128)[:, :, tsl], reads=[Bs_gT], writes=[Bgt_], own=Bgt_)
                for cc in range(8):
                    ba, bb = (g % 2) * 2, (g % 2) * 2 + 1
                    g += 1
                    for kc in range(8):
                        pe.op(lambda: T_.matmul(P[:, ba, :], lhsT=Wbs[:, kc, cc * 128:(cc + 1) * 128], rhs=ys_[:, kc, :],
                                                start=(kc == 0), stop=(kc == 7)), [BWbs, Bys_], [BP[ba]])
                    for kc in range(8):
                        pe.op(lambda: T_.matmul(P[:, bb, :], lhsT=Wba[:, kc, cc * 128:(cc + 1) * 128], rhs=ya_[:, kc, :],
                                                start=(kc == 0), stop=(kc == 7)), [BWba, Bya_], [BP[bb]])
                    dve.op(lambda: V_.tensor_tensor(out=m1[:], in0=P[:, ba, :], in1=gt_[:, cc, :], op=ALU.mult), [BP[ba], Bgt_], [Bm1])
                    dve.op(lambda: V_.tensor_tensor(out=m2[:], in0=P[:, bb, :], in1=gt_[:, 8 + cc, :], op=ALU.mult), [BP[bb], Bgt_], [Bm2])
                    pool.op(lambda: G_.tensor_tensor(out=mT[:, cc, :], in0=m1[:], in1=m2[:], op=ALU.add), [Bm1, Bm2], [BmT])
                for tt in range(4):
                    t = tb * 4 + tt
                    xo_, Bxo_ = xo[gx % 2]
                    gx += 1
                    sp.dma(xo_[:], src[t * 128:(t + 1) * 128, :], reads=[bsel(Bsrc, t)], writes=[Bxo_], own=Bxo_)
                    for dh in range(2):
                        bank = 4 + (2 * tt + dh) % 4
                        for cc in range(8):
                            pe.op(lambda: T_.matmul(P[:, bank, :], lhsT=mT[:, cc, tt * 128:(tt + 1) * 128],
                                                    rhs=Wo[:, cc, dh * 512:(dh + 1) * 512], start=(cc == 0), stop=(cc == 7)),
                                  [BmT, BWo], [BP[bank]])
                        dve.op(lambda: V_.tensor_tensor(out=xo_[:, dh * 512:(dh + 1) * 512], in0=P[:, bank, :],
                                                        in1=xo_[:, dh * 512:(dh + 1) * 512], op=ALU.add), [BP[bank], Bxo_], [Bxo_])
                    sp.dma(out[t * 128:(t + 1) * 128, :], xo_[:], reads=[Bxo_], writes=[Bout[t]], own=Bxo_)
            fw.barrier()
            fw.release([Bsnw, Bsw1] + [b for _, b in wfsE + ysb + yab + gtb + xo])
        stE.close()
        if stop == "E":
            break

        is_moe = (L % 2 == 1)
        jx = L // 2
        comb, Bcomb = sbuf(top, f"comb{L}", (128, NT, 8), F32)
        with ExitStack() as st:
            hT, _ = sbuf(st, "hT", (128, 8, S), BF16)
            BhT = [Buf(f"hT{t}") for t in range(NT)]
            hook = None
            if is_moe:
                nfc, Bnfc = col_load(st, "nfc", I["norm_ffn_w"][L], 8)
                rwf, Brwf = sbuf(st, "rwf", (128, 8, 8), F32)
                sp.dma(rwf[:], I["router_w"][jx].rearrange("(k p) e -> p k e", p=128), reads=[Bin], writes=[Brwf], own=Brwf)
                dve.op(lambda: V_.tensor_tensor(out=rwf[:], in0=rwf[:], in1=nfc[:].unsqueeze(2).broadcast_to([128, 8, 8]), op=ALU.mult),
                       [Brwf, Bnfc], [Brwf])
                xnf2 = [sbuf(st, f"xnfF{i}", (128, 1024), F32) for i in range(2)]
                xTf2 = [sbuf(st, f"xTf{i}", (128, 8, 128), F32) for i in range(2)]
                lgA, BlgA = sbuf(st, "lgA", (128, NT, 8), F32)

                def hook(t, x_, Bx, ss_, Bss):
                    (xnf, Bxnf), (xTf, BxTf) = xnf2[t % 2], xTf2[t % 2]
                    pb = 4 + 2 * (t % 2)
                    dve.op(lambda: V_.tensor_scalar(out=xnf[:], in0=x_[:], scalar1=ss_[:, 0:1], scalar2=None, op0=ALU.mult),
                           [Bx, Bss], [Bxnf])
                    for kc in range(8):
                        pe.op(lambda: T_.transpose(out=P[:, pb + kc // 4, (kc % 4) * 128:(kc % 4 + 1) * 128],
                                                   in_=xnf[:, kc * 128:(kc + 1) * 128], identity=IDf), [Bxnf, Bcm], [BP[pb + kc // 4]])
                    act.op(lambda: A_.copy(out=xTf[:].rearrange("p (a b) t -> p a (b t)", a=2), in_=P[:, pb:pb + 2, :]),
                           [BP[pb], BP[pb + 1]], [BxTf])
                    for kc in range(8):
                        pe.op(lambda: T_.matmul(P[:, pb, 0:8], lhsT=xTf[:, kc, :], rhs=rwf[:, kc, :], start=(kc == 0), stop=(kc == 7)),
                              [BxTf, Brwf], [BP[pb]])
                    dve.op(lambda: V_.tensor_copy(out=lgA[:, t, :], in_=P[:, pb, 0:8]), [BP[pb]], [BlgA])
            rel = norm_T(st, out, Bout, hT, BhT, "nF", hook)
            if is_moe:
                l2A, Bl2A = sbuf(st, "l2A", (128, NT, 8), F32)
                mk1A, Bmk1A = sbuf(st, "mk1A", (128, NT, 8), F32)
                mk2A, Bmk2A = sbuf(st, "mk2A", (128, NT, 8), F32)
                mxA, BmxA = sbuf(st, "mxA", (128, 4, NT), F32)
                fl = lambda ap: ap.rearrange("p t e -> p (t e)")
                bc = lambda ap: ap.unsqueeze(2).broadcast_to([128, NT, 8])
                dve.op(lambda: V_.tensor_reduce(out=mxA[:, 0, :], in_=lgA[:], axis=AX.X, op=ALU.max), [BlgA], [BmxA])
                dve.op(lambda: V_.tensor_tensor(out=mk1A[:], in0=lgA[:], in1=bc(mxA[:, 0, :]), op=ALU.is_equal), [BlgA, BmxA], [Bmk1A])
                dve.op(lambda: V_.scalar_tensor_tensor(out=fl(l2A[:]), in0=fl(mk1A[:]), scalar=-1e30, in1=fl(lgA[:]), op0=ALU.mult,
                                                       op1=ALU.add), [Bmk1A, BlgA], [Bl2A])
                dve.op(lambda: V_.tensor_reduce(out=mxA[:, 1, :], in_=l2A[:], axis=AX.X, op=ALU.max), [Bl2A], [BmxA])
                dve.op(lambda: V_.tensor_tensor(out=mk2A[:], in0=l2A[:], in1=bc(mxA[:, 1, :]), op=ALU.is_equal), [Bl2A, BmxA], [Bmk2A])
                dve.op(lambda: V_.tensor_tensor(out=mxA[:, 2, :], in0=mxA[:, 0, :], in1=mxA[:, 1, :], op=ALU.subtract), [BmxA], [BmxA])
                act.op(lambda: A_.activation(out=mxA[:, 3, :], in_=mxA[:, 2, :], func=AF.Sigmoid, scale=-1.0), [BmxA], [BmxA])
                act.op(lambda: A_.activation(out=mxA[:, 2, :], in_=mxA[:, 2, :], func=AF.Sigmoid), [BmxA], [BmxA])
                dve.op(lambda: V_.tensor_tensor(out=mk1A[:], in0=mk1A[:], in1=bc(mxA[:, 2, :]), op=ALU.mult), [Bmk1A, BmxA], [Bmk1A])
                dve.op(lambda: V_.tensor_tensor(out=mk2A[:], in0=mk2A[:], in1=bc(mxA[:, 3, :]), op=ALU.mult), [Bmk2A, BmxA], [Bmk2A])
                dve.op(lambda: V_.tensor_tensor(out=comb[:], in0=mk2A[:], in1=mk1A[:], op=ALU.add), [Bmk2A, Bmk1A], [Bcomb])
            hTo, BhTo = sbuf(st, "hTo", (1, 2), F32)
            for kc in range(8):
                sp.dma(s_hT[kc * 128:(kc + 1) * 128, :], hT[:, kc, :], reads=BhT, writes=[Bs_hT], own=BhTo)
            fw.barrier()
            fw.release(rel + [BhTo] + ([Bnfc, Brwf] if is_moe else []))

        FB = min(1024, S)
        NFB = S // FB
        with ExitStack() as st:
            wfs = [sbuf(st, f"wfF{i}", (128, 8, 512), F32) for i in range(2)]
            wbs = [sbuf(st, f"wbF{i}", (128, 8, 512), BF16) for i in range(4)]
            nfc, Bnfc = col_load(st, "nfc2", I["norm_ffn_w"][L], 8)
            Wd, BWd = sbuf(st, "Wd", (128, NFF, 1024), BF16)
            HT, BHT = sbuf(st, "HT", (128, NFF, FB), BF16)
            hb, Bhb = sbuf(st, "hb", (128, 8, FB), BF16)
            sg, Bsg = sbuf(st, "sg", (128, 512), F32)
            xo = [sbuf(st, f"xoF{i}", (128, 1024), F32) for i in range(2)]
            gx = 0
            gw = 0
            gp = 0
            experts = list(range(NEXP)) if is_moe else [None]

            def w_aps(e):
                if is_moe:
                    return I["moe_w_gate"][jx][e], I["moe_w_up"][jx][e], I["moe_w_down"][jx][e]
                return I["ffn_w_gate"][jx], I["ffn_w_up"][jx], I["ffn_w_down"][jx]
            groups = [(e, fb, f0) for e in experts for fb in range(NFB) for f0 in range(0, NFF, 4)]
            gidx = {g_: i for i, g_ in enumerate(groups)}

            def issue_group(i):
                nonlocal_gw = rotw
                e_, fb_, f0_ = groups[i]
                nf_ = min(4, NFF - f0_)
                Wg_x, Wu_x, _ = w_aps(e_)
                res_ = []
                for W_a in (Wg_x, Wu_x):
                    wb_, Bwb_ = wbs[nonlocal_gw[0] % 4]
                    nonlocal_gw[0] += 1
                    cast_load(wfs, W_a[:, f0_ * 128:(f0_ + nf_) * 128].rearrange("(k p) c -> p k c", p=128), 8, nf_ * 128,
                              nfc[:, :], Bnfc, wb_[:, :, :nf_ * 128], Bwb_)
                    res_.append((wb_, Bwb_))
                return res_
            rotw = [0]
            pendg = {}
            for e in experts:
                Wg_a, Wu_a, Wd_a = w_aps(e)
                wd_jobs = [(f0, min(8, NFF - f0), hf) for f0 in range(0, NFF, 8) for hf in range(2)]
                for fb in range(NFB):
                    sp.dma(hb[:], s_hT.rearrange("(k p) s -> p k s", p=128)[:, :, fb * FB:(fb + 1) * FB], reads=[Bs_hT], writes=[Bhb], own=Bhb)
                    for f0 in range(0, NFF, 4):
                        nf = min(4, NFF - f0)
                        gi_ = gidx[(e, fb, f0)]
                        wgu = pendg.pop(gi_) if gi_ in pendg else issue_group(gi_)
                        if gi_ + 1 < len(groups):
                            pendg[gi_ + 1] = issue_group(gi_ + 1)
                        if fb == 0:
                            for _ in range(1 if f0 // 4 < 2 else 2):
                                if wd_jobs and f0 // 4 >= 1:
                                    wf0, wnk, whf = wd_jobs.pop(0)
                                    cast_load(wfs, Wd_a[wf0 * 128:(wf0 + wnk) * 128, whf * 512:(whf + 1) * 512].rearrange("(k p) c -> p k c", p=128),
                                              wnk, 512, None, None, Wd[:, wf0:wf0 + wnk, whf * 512:(whf + 1) * 512], BWd)
                        for fi in range(nf):
                            f = f0 + fi
                            for t5 in range(FB // 512):
                                bg, bu = (gp % 2) * 2, (gp % 2) * 2 + 1
                                gp += 1
                                for (wb_, Bwb_), bk in zip(wgu, (bg, bu)):
                                    for kc in range(8):
                                        pe.op(lambda: T_.matmul(P[:, bk, :], lhsT=wb_[:, kc, fi * 128:(fi + 1) * 128],
                                                                rhs=hb[:, kc, t5 * 512:(t5 + 1) * 512], start=(kc == 0), stop=(kc == 7)),
                                              [Bwb_, Bhb], [BP[bk]])
                                act.op(lambda: A_.activation(out=sg[:], in_=P[:, bg, :], func=AF.Silu), [BP[bg]], [Bsg])
                                dve.op(lambda: V_.tensor_tensor(out=HT[:, f, t5 * 512:(t5 + 1) * 512], in0=P[:, bu, :], in1=sg[:], op=ALU.mult),
                                       [BP[bu], Bsg], [BHT])
                    while fb == 0 and wd_jobs:
                        wf0, wnk, whf = wd_jobs.pop(0)
                        cast_load(wfs, Wd_a[wf0 * 128:(wf0 + wnk) * 128, whf * 512:(whf + 1) * 512].rearrange("(k p) c -> p k c", p=128),
                                  wnk, 512, None, None, Wd[:, wf0:wf0 + wnk, whf * 512:(whf + 1) * 512], BWd)
                    for tt in range(FB // 128):
                        t = fb * (FB // 128) + tt
                        xo_, Bxo_ = xo[gx % 2]
                        gx += 1
                        sp.dma(xo_[:], out[t * 128:(t + 1) * 128, :], reads=[Bout[t]], writes=[Bxo_], own=Bxo_)
                        for dh in range(2):
                            bank = 4 + (2 * tt + dh) % 4
                            for f in range(NFF):
                                pe.op(lambda: T_.matmul(P[:, bank, :], lhsT=HT[:, f, tt * 128:(tt + 1) * 128],
                                                        rhs=Wd[:, f, dh * 512:(dh + 1) * 512], start=(f == 0), stop=(f == NFF - 1)),
                                      [BHT, BWd], [BP[bank]])
                            xs_ = xo_[:, dh * 512:(dh + 1) * 512]
                            if is_moe:
                                dve.op(lambda: V_.scalar_tensor_tensor(out=xs_, in0=P[:, bank, :], scalar=comb[:, t, e:e + 1], in1=xs_,
                                                                       op0=ALU.mult, op1=ALU.add), [BP[bank], Bcomb, Bxo_], [Bxo_])
                            else:
                                dve.op(lambda: V_.tensor_tensor(out=xs_, in0=P[:, bank, :], in1=xs_, op=ALU.add), [BP[bank], Bxo_], [Bxo_])
                        sp.dma(out[t * 128:(t + 1) * 128, :], xo_[:], reads=[Bxo_], writes=[Bout[t]], own=Bxo_)
            fw.barrier()
            fw.release([Bnfc, Bhb] + [b for _, b in wfs + xo])

    fw.barrier()
    top.close()
    fw.stack.close()
    return nc


def kernel(**inputs):
    S = inputs["x"].shape[1]
    nc = build(S=S)
    consts = host_consts(S)
    in_maps = []
    for b in range(8):
        m = {"x": np.ascontiguousarray(inputs["x"][b])}
        for k in W_SHAPES:
            m[k] = np.ascontiguousarray(inputs[k])
        m.update(consts)
        in_maps.append(m)
    res = run_bass_kernel_spmd(nc, in_maps, core_ids=list(range(8)))
    return np.stack([r["out"] for r in res.results], 0)
```
